# Optimizing a Trainium2 kernel written in Bass

```python
import numpy as np
import jax
import jax.numpy as jnp
from jax import lax

D_MODEL = 4096
BATCH = 4
SEQ = 4096
DEPTH = 2

N_MOD = 6
RMS_EPS = 1e-6
CONV_WIDTH = 1024
CONV_K = 3
NSA_HEADS = 16
NSA_KV_GROUPS = 4
NSA_HEAD_DIM = 128
CMP_BLOCK = 32
CMP_STRIDE = 16
CMP_HIDDEN = 128
SEL_BLOCK = 64
N_SELECT = 16
WINDOW = 512
SEL_Q_CHUNK = 32
WIN_Q_BLOCK = 128
FORCE_SCORE = 1e9
NEG_INF = -1e30
ROPE_THETA = 500000.0
ROPE_DIMS = NSA_HEAD_DIM // 4
NSA_Q_WIDTH = NSA_HEADS * NSA_HEAD_DIM
NSA_KV_WIDTH = 6 * NSA_KV_GROUPS * NSA_HEAD_DIM
NSA_GATE_WIDTH = 3 * NSA_HEADS
RWKV_HEADS = 16
RWKV_HEAD_DIM = 64
RWKV_WIDTH = RWKV_HEADS * RWKV_HEAD_DIM
RWKV_DECAY_LORA = 64
RWKV_A_LORA = 64
RWKV_GATE_LORA = 160
RWKV_IN_WIDTH = 3 * RWKV_WIDTH + RWKV_DECAY_LORA + RWKV_A_LORA + RWKV_GATE_LORA
RWKV_LN_EPS = 64e-5
N_BRANCHES = 3
MIX_WIDTH = CONV_WIDTH + NSA_Q_WIDTH + RWKV_WIDTH
IN_WIDTH = 3 * CONV_WIDTH + NSA_Q_WIDTH + NSA_KV_WIDTH + NSA_GATE_WIDTH + RWKV_IN_WIDTH + N_BRANCHES * D_MODEL
N_EXPERTS = 32
TOP_K = 4
EXPERT_FF = 512
SWIGLU_ALPHA = 1.702
SWIGLU_LIMIT = 7.0
MOE_BLOCK = 128

kernel_name = 'hybrid_conv_nsa_rwkv7_moe_adaln'


def rms_norm(x, g):
    xf = x.astype(jnp.float32)
    y = xf * lax.rsqrt(jnp.mean(xf * xf, axis=-1, keepdims=True) + RMS_EPS)
    return (y * g.astype(jnp.float32)).astype(x.dtype)


def rope_tables(seq):
    inv = ROPE_THETA ** (-jnp.arange(0, ROPE_DIMS, 2, dtype=jnp.float32) / ROPE_DIMS)
    ang = jnp.arange(seq, dtype=jnp.float32)[:, None] * inv[None, :]
    return jnp.cos(ang), jnp.sin(ang)


def apply_rope(x, cos, sin):
    half = ROPE_DIMS // 2
    c = cos[None, :, None, :].astype(x.dtype)
    s = sin[None, :, None, :].astype(x.dtype)
    x1 = x[..., :half]
    x2 = x[..., half:ROPE_DIMS]
    return jnp.concatenate([x1 * c - x2 * s, x2 * c + x1 * s, x[..., ROPE_DIMS:]], axis=-1)


def causal_dwconv(u, w):
    return lax.conv_general_dilated(
        u, w[:, None, :].astype(u.dtype), window_strides=(1,),
        padding=[(CONV_K - 1, 0)], dimension_numbers=('NWC', 'WIO', 'NWC'),
        feature_group_count=u.shape[-1])


def compress_blocks(t, pe, w1, w2):
    Bb, S, G, Dh = t.shape
    r = CMP_BLOCK // CMP_STRIDE
    n_sub = S // CMP_STRIDE
    nc = n_sub - r + 1
    sub = t.reshape(Bb, n_sub, CMP_STRIDE, G, Dh)
    blocks = jnp.concatenate([sub[:, i:i + nc] for i in range(r)], axis=2)
    blocks = blocks + pe[None, None, :, None, :]
    flat = blocks.transpose(0, 1, 3, 2, 4).reshape(Bb, nc, G, CMP_BLOCK * Dh)
    return jax.nn.gelu(flat @ w1) @ w2


def block_map(nc, nb):
    i = np.arange(nc)[:, None] * CMP_STRIDE
    j = np.arange(nb)[None, :] * SEL_BLOCK
    inter = np.clip(np.minimum(i + CMP_BLOCK, j + SEL_BLOCK) - np.maximum(i, j), 0, None)
    return jnp.asarray(inter.astype(np.float32) / np.float32(CMP_BLOCK))


def nsa_attention(q, kc, vc, ks, vs, kw, vw, gates, cmp_pe, cmp_w1, cmp_w2):
    Bb, S, H, Dh = q.shape
    G = kc.shape[2]
    hpg = H // G
    f32 = jnp.float32
    scale = Dh ** -0.5
    qg = q.reshape(Bb, S, G, hpg, Dh)
    t = jnp.arange(S)

    kcmp = compress_blocks(kc, cmp_pe[0], cmp_w1[0], cmp_w2[0])
    vcmp = compress_blocks(vc, cmp_pe[1], cmp_w1[1], cmp_w2[1])
    nc = kcmp.shape[1]
    s_c = jnp.einsum('bsghd,bngd->bghsn', qg, kcmp, preferred_element_type=f32) * scale
    valid_c = (jnp.arange(nc) * CMP_STRIDE + CMP_BLOCK - 1)[None, :] <= t[:, None]
    p_c = jax.nn.softmax(jnp.where(valid_c, s_c, NEG_INF), axis=-1)
    p_c = jnp.where(valid_c, p_c, 0.0)
    o_c = jnp.einsum('bghsn,bngd->bsghd', p_c.astype(vcmp.dtype), vcmp)

    nb = S // SEL_BLOCK
    imp = jnp.einsum('bghsn,nj->bgsj', p_c, block_map(nc, nb))
    jb = jnp.arange(nb)[None, :]
    cur = (t // SEL_BLOCK)[:, None]
    forced = (jb == 0) | (jb == cur) | (jb == cur - 1)
    imp = jnp.where(forced, FORCE_SCORE, imp)
    imp = jnp.where(jb <= cur, imp, NEG_INF)
    n_sel = min(N_SELECT, nb)
    _, sel_idx = lax.top_k(imp, n_sel)

    kb = ks.reshape(Bb, nb, SEL_BLOCK, G, Dh).transpose(0, 3, 1, 2, 4)
    vb = vs.reshape(Bb, nb, SEL_BLOCK, G, Dh).transpose(0, 3, 1, 2, 4)
    nq = S // SEL_Q_CHUNK
    q_ch = qg.reshape(Bb, nq, SEL_Q_CHUNK, G, hpg, Dh).transpose(1, 0, 2, 3, 4, 5)
    idx_ch = sel_idx.reshape(Bb, G, nq, SEL_Q_CHUNK, n_sel).transpose(2, 0, 1, 3, 4)
    t_ch = t.reshape(nq, SEL_Q_CHUNK)
    bi = jnp.arange(Bb)[:, None, None, None]
    gi = jnp.arange(G)[None, :, None, None]

    def sel_chunk(args):
        qc, ic, tc = args
        k_sel = kb[bi, gi, ic]
        v_sel = vb[bi, gi, ic]
        s = jnp.einsum('bqghd,bgqnld->bghqnl', qc, k_sel, preferred_element_type=f32) * scale
        kpos = ic[..., None] * SEL_BLOCK + jnp.arange(SEL_BLOCK)
        mask = kpos <= tc[None, None, :, None, None]
        s = jnp.where(mask[:, :, None], s, NEG_INF)
        p = jax.nn.softmax(s.reshape(Bb, G, hpg, SEL_Q_CHUNK, n_sel * SEL_BLOCK), axis=-1)
        p = p.reshape(Bb, G, hpg, SEL_Q_CHUNK, n_sel, SEL_BLOCK)
        return jnp.einsum('bghqnl,bgqnld->bqghd', p.astype(v_sel.dtype), v_sel)

    o_s = lax.map(sel_chunk, (q_ch, idx_ch, t_ch))
    o_s = o_s.transpose(1, 0, 2, 3, 4, 5).reshape(Bb, S, G, hpg, Dh)

    kwp = jnp.pad(kw, ((0, 0), (WINDOW, 0), (0, 0), (0, 0)))
    vwp = jnp.pad(vw, ((0, 0), (WINDOW, 0), (0, 0), (0, 0)))
    nqb = S // WIN_Q_BLOCK
    span = WINDOW + WIN_Q_BLOCK
    q_blk = qg.reshape(Bb, nqb, WIN_Q_BLOCK, G, hpg, Dh).transpose(1, 0, 2, 3, 4, 5)

    def win_block(args):
        qb, b = args
        start = b * WIN_Q_BLOCK
        kblk = lax.dynamic_slice_in_dim(kwp, start, span, axis=1)
        vblk = lax.dynamic_slice_in_dim(vwp, start, span, axis=1)
        s = jnp.einsum('bqghd,bkgd->bghqk', qb, kblk, preferred_element_type=f32) * scale
        qpos = start + jnp.arange(WIN_Q_BLOCK)
        kpos = start - WINDOW + jnp.arange(span)
        diff = qpos[:, None] - kpos[None, :]
        mask = (diff >= 0) & (diff < WINDOW) & (kpos[None, :] >= 0)
        p = jax.nn.softmax(jnp.where(mask, s, NEG_INF), axis=-1)
        return jnp.einsum('bghqk,bkgd->bqghd', p.astype(vblk.dtype), vblk)

    o_w = lax.map(win_block, (q_blk, jnp.arange(nqb)))
    o_w = o_w.transpose(1, 0, 2, 3, 4, 5).reshape(Bb, S, G, hpg, Dh)

    g = gates.reshape(Bb, S, G, hpg, 3)
    o = g[..., 0:1] * o_c + g[..., 1:2] * o_s + g[..., 2:3] * o_w
    return o.reshape(Bb, S, H * Dh)


def rwkv7_time_mix(u, mu, w0, wb, a0, ab, gb, kk_scale, ka, rk, ln_w, ln_b):
    Bb, S, _ = u.shape
    H, N, W = RWKV_HEADS, RWKV_HEAD_DIM, RWKV_WIDTH
    f32 = jnp.float32
    u_prev = jnp.pad(u, ((0, 0), (1, 0), (0, 0)))[:, :-1]
    um = u + (u_prev - u) * mu
    r, k, v, lw, la, lg = jnp.split(
        um, [W, 2 * W, 3 * W, 3 * W + RWKV_DECAY_LORA, 3 * W + RWKV_DECAY_LORA + RWKV_A_LORA], axis=-1)
    w = -jax.nn.softplus(-(w0 + jnp.tanh(lw) @ wb).astype(f32)) - 0.5
    decay = jnp.exp(-jnp.exp(w))
    a = jax.nn.sigmoid((a0 + la @ ab).astype(f32))
    g = jax.nn.sigmoid(lg) @ gb

    def heads(z):
        return z.astype(f32).reshape(Bb, S, H, N)

    r, k, v, decay, a = heads(r), heads(k), heads(v), heads(decay), heads(a)
    kk = k * kk_scale.astype(f32).reshape(H, N)
    kk = kk / jnp.maximum(jnp.sqrt(jnp.sum(kk * kk, axis=-1, keepdims=True)), 1e-12)
    k = k * (1.0 + (a - 1.0) * ka.astype(f32).reshape(H, N))

    def step(state, inp):
        r_t, w_t, k_t, v_t, kk_t, a_t = inp
        sa = jnp.einsum('bhvk,bhk->bhv', state, -kk_t)
        state = (state * w_t[:, :, None, :]
                 + sa[..., None] * (kk_t * a_t)[:, :, None, :]
                 + v_t[..., None] * k_t[:, :, None, :])
        return state, jnp.einsum('bhvk,bhk->bhv', state, r_t)

    xs = tuple(jnp.moveaxis(z, 1, 0) for z in (r, decay, k, v, kk, a))
    _, o = lax.scan(step, jnp.zeros((Bb, H, N, N), f32), xs)
    o = jnp.moveaxis(o, 0, 1)
    mean = jnp.mean(o, axis=-1, keepdims=True)
    var = jnp.mean(jnp.square(o - mean), axis=-1, keepdims=True)
    o = (o - mean) * lax.rsqrt(var + RWKV_LN_EPS)
    o = o * ln_w.astype(f32).reshape(H, N) + ln_b.astype(f32).reshape(H, N)
    o = o + jnp.sum(r * k * rk.astype(f32), axis=-1, keepdims=True) * v
    return (o.reshape(Bb, S, W) * g.astype(f32)).astype(u.dtype)


def hybrid_mixer(h, w_in, conv_w, cmp_pe, cmp_w1, cmp_w2, rwkv_mu, rwkv_w0, rwkv_wb, rwkv_a0,
                 rwkv_ab, rwkv_gb, rwkv_kk, rwkv_ka, rwkv_rk, rwkv_ln_w, rwkv_ln_b,
                 w_branch, w_out, cos, sin):
    Bb, S, D = h.shape
    z = h @ w_in
    offs = [int(o) for o in np.cumsum(
        [3 * CONV_WIDTH, NSA_Q_WIDTH, NSA_KV_WIDTH, NSA_GATE_WIDTH, RWKV_IN_WIDTH])]
    z_conv, z_q, z_kv, z_g, z_rwkv, z_merge = jnp.split(z, offs, axis=-1)

    cb, cc, ch = jnp.split(z_conv, 3, axis=-1)
    y_a = cb * causal_dwconv(cc * ch, conv_w)

    q = apply_rope(z_q.reshape(Bb, S, NSA_HEADS, NSA_HEAD_DIM), cos, sin)
    kv = z_kv.reshape(Bb, S, 6, NSA_KV_GROUPS, NSA_HEAD_DIM)
    kc = apply_rope(kv[:, :, 0], cos, sin)
    ks = apply_rope(kv[:, :, 2], cos, sin)
    kw = apply_rope(kv[:, :, 4], cos, sin)
    gates = jax.nn.sigmoid(z_g.reshape(Bb, S, NSA_HEADS, 3))
    y_b = nsa_attention(q, kc, kv[:, :, 1], ks, kv[:, :, 3], kw, kv[:, :, 5], gates,
                        cmp_pe, cmp_w1, cmp_w2)

    y_c = rwkv7_time_mix(z_rwkv, rwkv_mu, rwkv_w0, rwkv_wb, rwkv_a0, rwkv_ab, rwkv_gb,
                         rwkv_kk, rwkv_ka, rwkv_rk, rwkv_ln_w, rwkv_ln_b)

    gm = jax.nn.sigmoid(z_merge.reshape(Bb, S, N_BRANCHES, D))
    pa, pb, pc = jnp.split(w_branch, [CONV_WIDTH, CONV_WIDTH + NSA_Q_WIDTH], axis=0)
    m = gm[:, :, 0] * (y_a @ pa) + gm[:, :, 1] * (y_b @ pb) + gm[:, :, 2] * (y_c @ pc)
    return m @ w_out


def clamped_swiglu(g, u):
    g = jnp.minimum(g, SWIGLU_LIMIT)
    u = jnp.clip(u, -SWIGLU_LIMIT, SWIGLU_LIMIT)
    return g * jax.nn.sigmoid(SWIGLU_ALPHA * g) * (u + 1.0)


def moe_ffn(h, router_w, router_b, wg, bg, wu, bu, wd, bd):
    Bb, S, D = h.shape
    T = Bb * S
    A = T * TOP_K
    f32 = jnp.float32
    ht = h.reshape(T, D)
    logits = jnp.dot(ht, router_w, preferred_element_type=f32) + router_b.astype(f32)
    top_val, top_idx = lax.top_k(logits, TOP_K)
    top_w = jax.nn.softmax(top_val, axis=-1)
    flat_e = top_idx.reshape(A)
    flat_tok = jnp.arange(A, dtype=jnp.int32) // TOP_K
    flat_w = top_w.reshape(A)
    order = jnp.argsort(flat_e)
    sorted_e = flat_e[order]
    counts = jnp.bincount(flat_e, length=N_EXPERTS)
    starts = jnp.cumsum(counts) - counts
    padded = (counts + MOE_BLOCK - 1) // MOE_BLOCK * MOE_BLOCK
    pends = jnp.cumsum(padded)
    pstarts = pends - padded
    dest = pstarts[sorted_e] + jnp.arange(A, dtype=jnp.int32) - starts[sorted_e]
    n_blk = -(-(A + N_EXPERTS * (MOE_BLOCK - 1)) // MOE_BLOCK)
    P = n_blk * MOE_BLOCK
    row_tok = jnp.full((P,), T, jnp.int32).at[dest].set(flat_tok[order])
    row_w = jnp.zeros((P,), f32).at[dest].set(flat_w[order])
    blk_e = jnp.minimum(jnp.searchsorted(pends, jnp.arange(n_blk) * MOE_BLOCK, side='right'),
                        N_EXPERTS - 1)
    ht_pad = jnp.concatenate([ht, jnp.zeros((1, D), ht.dtype)], axis=0)

    def body(acc, blk):
        toks, wts, e = blk
        xb = ht_pad[toks]
        y = clamped_swiglu(xb @ wg[e] + bg[e], xb @ wu[e] + bu[e]) @ wd[e] + bd[e]
        return acc.at[toks].add(y.astype(f32) * wts[:, None]), None

    acc, _ = lax.scan(body, jnp.zeros((T + 1, D), f32),
                      (row_tok.reshape(n_blk, MOE_BLOCK), row_w.reshape(n_blk, MOE_BLOCK), blk_e))
    return acc[:T].reshape(Bb, S, D).astype(h.dtype)


def setup_inputs(seed: int = 0) -> dict:
    key = jax.random.key(seed)
    ks = jax.random.split(key, 40)
    f32 = jnp.float32
    L, D, E, F = DEPTH, D_MODEL, N_EXPERTS, EXPERT_FF

    def nrm(i, shape, scale):
        return jax.random.normal(ks[i], shape, f32) * scale

    def uni(i, shape, lo, hi):
        return jax.random.uniform(ks[i], shape, f32, lo, hi)

    return {
        'x': nrm(0, (BATCH, SEQ, D), 1.0),
        'c': nrm(1, (BATCH, D), 1.0),
        'ada_w': nrm(2, (D, N_MOD * D), 0.25 * D ** -0.5),
        'ada_b': nrm(3, (N_MOD * D,), 0.02),
        'ada_table': nrm(4, (L, N_MOD, D), 0.1),
        'norm_g': 1.0 + nrm(5, (L, 2, D), 0.05),
        'final_g': 1.0 + nrm(6, (D,), 0.05),
        'w_in': nrm(7, (L, D, IN_WIDTH), D ** -0.5),
        'conv_w': nrm(8, (L, CONV_K, CONV_WIDTH), CONV_K ** -0.5),
        'cmp_pe': nrm(9, (L, 2, CMP_BLOCK, NSA_HEAD_DIM), 0.1),
        'cmp_w1': nrm(10, (L, 2, CMP_BLOCK * NSA_HEAD_DIM, CMP_HIDDEN), (CMP_BLOCK * NSA_HEAD_DIM) ** -0.5),
        'cmp_w2': nrm(11, (L, 2, CMP_HIDDEN, NSA_HEAD_DIM), CMP_HIDDEN ** -0.5),
        'rwkv_mu': uni(12, (L, RWKV_IN_WIDTH), 0.0, 1.0),
        'rwkv_w0': uni(13, (L, RWKV_WIDTH), -6.0, -1.0),
        'rwkv_wb': nrm(14, (L, RWKV_DECAY_LORA, RWKV_WIDTH), 0.5 * RWKV_DECAY_LORA ** -0.5),
        'rwkv_a0': nrm(15, (L, RWKV_WIDTH), 0.5),
        'rwkv_ab': nrm(16, (L, RWKV_A_LORA, RWKV_WIDTH), 0.5 * RWKV_A_LORA ** -0.5),
        'rwkv_gb': nrm(17, (L, RWKV_GATE_LORA, RWKV_WIDTH), RWKV_GATE_LORA ** -0.5),
        'rwkv_kk': 0.85 + nrm(18, (L, RWKV_WIDTH), 0.05),
        'rwkv_ka': 1.0 + nrm(19, (L, RWKV_WIDTH), 0.05),
        'rwkv_rk': nrm(20, (L, RWKV_HEADS, RWKV_HEAD_DIM), 0.1),
        'rwkv_ln_w': 1.0 + nrm(21, (L, RWKV_WIDTH), 0.05),
        'rwkv_ln_b': nrm(22, (L, RWKV_WIDTH), 0.02),
        'w_branch': nrm(23, (L, MIX_WIDTH, D), CONV_WIDTH ** -0.5),
        'w_out': nrm(24, (L, D, D), D ** -0.5),
        'router_w': nrm(25, (L, D, E), D ** -0.5),
        'router_b': nrm(26, (L, E), 0.01),
        'exp_wg': nrm(27, (L, E, D, F), D ** -0.5),
        'exp_bg': nrm(28, (L, E, F), 0.01),
        'exp_wu': nrm(29, (L, E, D, F), D ** -0.5),
        'exp_bu': nrm(30, (L, E, F), 0.01),
        'exp_wd': nrm(31, (L, E, F, D), F ** -0.5),
        'exp_bd': nrm(32, (L, E, D), 0.01),
    }


def reference(x, c, ada_w, ada_b, ada_table, norm_g, final_g, w_in, conv_w, cmp_pe, cmp_w1,
              cmp_w2, rwkv_mu, rwkv_w0, rwkv_wb, rwkv_a0, rwkv_ab, rwkv_gb, rwkv_kk, rwkv_ka,
              rwkv_rk, rwkv_ln_w, rwkv_ln_b, w_branch, w_out, router_w, router_b, exp_wg,
              exp_bg, exp_wu, exp_bu, exp_wd, exp_bd):
    Bb, S, D = x.shape
    cos, sin = rope_tables(S)
    mod_all = (jax.nn.silu(c) @ ada_w + ada_b).reshape(Bb, N_MOD, D)
    for l in range(DEPTH):
        mod = mod_all + ada_table[l]
        shift1 = mod[:, 0, None, :]
        scale1 = mod[:, 1, None, :]
        gate1 = mod[:, 2, None, :]
        shift2 = mod[:, 3, None, :]
        scale2 = mod[:, 4, None, :]
        gate2 = mod[:, 5, None, :]
        h = rms_norm(x, norm_g[l, 0]) * (1.0 + scale1) + shift1
        x = x + gate1 * hybrid_mixer(
            h, w_in[l], conv_w[l], cmp_pe[l], cmp_w1[l], cmp_w2[l], rwkv_mu[l], rwkv_w0[l],
            rwkv_wb[l], rwkv_a0[l], rwkv_ab[l], rwkv_gb[l], rwkv_kk[l], rwkv_ka[l], rwkv_rk[l],
            rwkv_ln_w[l], rwkv_ln_b[l], w_branch[l], w_out[l], cos, sin)
        h = rms_norm(x, norm_g[l, 1]) * (1.0 + scale2) + shift2
        x = x + gate2 * moe_ffn(h, router_w[l], router_b[l], exp_wg[l], exp_bg[l], exp_wu[l],
                                exp_bu[l], exp_wd[l], exp_bd[l])
    return rms_norm(x, final_g)
```

```python
import numpy as np
import concourse.bass as bass
import concourse.mybir as mybir
from concourse.bass_utils import run_bass_kernel_spmd

F32 = mybir.dt.float32
AF = mybir.ActivationFunctionType
ALU = mybir.AluOpType
AX = mybir.AxisListType


class Dep:
    __slots__ = ("w", "r", "name", "excl")

    def __init__(self, name=""):
        self.w = None
        self.r = []
        self.name = name
        self.excl = False


class Buf(Dep):
    __slots__ = ("t", "subs")

    def __init__(self, t, name=""):
        super().__init__(name)
        self.t = t
        self.subs = {}

    def __getitem__(self, idx):
        return self.t[idx]

    def d(self, key):
        if self.excl:
            return self
        s = self.subs.get(key)
        if s is None:
            s = Dep(f"{self.name}.{key}")
            self.subs[key] = s
        return s


class MK:
    COMPUTE = ("pe", "act", "dve", "pool")

    def __init__(self, nc, n_dma_sems=12, same_engine_sync=True):
        self.nc = nc
        self.same_engine_sync = same_engine_sync
        self.ops = {k: [] for k in ("pe", "act", "dve", "pool", "sp")}
        self.seq = {k: 0 for k in self.COMPUTE}
        self.waited = {k: {} for k in self.ops}
        self._stack = []
        self.esem = {}
        for k in self.COMPUTE:
            self.esem[k] = self._enter(nc.semaphore(f"s_{k}"))
        self.dsem = {}
        self.drr = {}
        for q in ("sp", "pool", "act"):
            self.dsem[q] = [[self._enter(nc.semaphore(f"d_{q}{i}")), 0] for i in range(n_dma_sems)]
            self.drr[q] = 0
        self.psum_banks = []
        self.n_instr = 0
        import os as _os
        self.cut = int(_os.environ["MK_CUT"]) if _os.environ.get("MK_CUT") else None
        self.log = [] if _os.environ.get("MK_LOG") else None

    def _enter(self, cm):
        v = cm.__enter__()
        self._stack.append(cm)
        return v

    def sb(self, name, shape, dtype=F32):
        self._uid = getattr(self, "_uid", 0) + 1
        t = self._enter(self.nc.sbuf_tensor(f"sb_{name}_{self._uid}", list(shape), dtype))
        return Buf(t, name)

    def ps(self, name, shape, dtype=F32):
        self._uid = getattr(self, "_uid", 0) + 1
        t = self._enter(self.nc.psum_tensor(f"ps_{name}_{self._uid}", list(shape), dtype))
        b = Buf(t, name)
        b.excl = True
        return b

    def dram(self, name, shape, dtype=F32, kind="Internal"):
        t = self.nc.dram_tensor(name, list(shape), dtype, kind=kind)
        return Buf(t, name)

    def _deps(self, reads, writes):
        evs = []
        for b in reads:
            if b.w is not None:
                evs.append(b.w)
            if b.excl:
                evs.extend(b.r)
        for b in writes:
            if b.w is not None:
                evs.append(b.w)
            evs.extend(b.r)
        return evs

    def _waits_for(self, q, evs):
        need = {}
        for ev in evs:
            if ev[0] == "c":
                _, e, s = ev
                if e == q:
                    if e == "pe" or not self.same_engine_sync:
                        continue
                key = ("c", e)
                sem = self.esem[e]
                val = s
            else:
                _, dq, i, val = ev
                key = ("d", dq, i)
                sem = self.dsem[dq][i][0]
            if self.waited[q].get(key, 0) >= val:
                continue
            if key not in need or need[key][1] < val:
                need[key] = (sem, val)
        out = []
        for key, (sem, val) in need.items():
            self.waited[q][key] = val
            out.append((sem, val))
        return out

    def op(self, q, fn, reads=(), writes=()):
        if getattr(self, "cut", None) is not None and self.n_instr >= self.cut:
            return None
        evs = self._deps(reads, writes)
        waits = self._waits_for(q, evs)
        self.seq[q] += 1
        ev = ("c", q, self.seq[q])
        self.ops[q].append((waits, fn, (self.esem[q], 1)))
        for b in reads:
            if b.excl:
                b.w = ev
                b.r = []
            else:
                b.r.append(ev)
        for b in writes:
            b.w = ev
            b.r = []
        self.n_instr += 1
        return ev

    def dma(self, q, out, in_, reads=(), writes=(), **kw):
        if getattr(self, "cut", None) is not None and self.n_instr >= self.cut:
            return None
        evs = self._deps(reads, writes)
        pool = self.dsem[q]
        i = self.drr[q]
        self.drr[q] = (i + 1) % len(pool)
        sem, uses = pool[i]
        if uses > 0:
            evs.append(("d", q, i, 16 * uses))
        waits = self._waits_for(q, evs)
        pool[i][1] = uses + 1
        ev = ("d", q, i, 16 * (uses + 1))

        def fn(e, out=out, in_=in_, kw=kw):
            return e.dma_start(out=out, in_=in_, **kw)

        self.ops[q].append((waits, fn, (sem, 16)))
        for b in reads:
            if b.excl:
                b.w = ev
                b.r = []
            else:
                b.r.append(ev)
        for b in writes:
            b.w = ev
            b.r = []
        self.n_instr += 1
        return ev

    def finish(self, final_deps=()):
        evs = []
        for b in final_deps:
            if b.w is not None:
                evs.append(b.w)
        waits = self._waits_for("sp", evs)
        self.ops["sp"].append((waits, None, None))
        nc = self.nc
        ops = self.ops

        def emit(e, lst):
            for waits, fn, inc in lst:
                for sem, val in waits:
                    e.wait_ge(sem, val)
                if fn is not None:
                    fn(e).then_inc(inc[0], inc[1])

        with nc.Block() as block:
            @block.tensor
            def _(e):
                emit(e, ops["pe"])

            @block.scalar
            def _(e):
                emit(e, ops["act"])

            @block.vector
            def _(e):
                emit(e, ops["dve"])

            @block.gpsimd
            def _(e):
                emit(e, ops["pool"])

            @block.sync
            def _(e):
                emit(e, ops["sp"])
        for cm in reversed(self._stack):
            cm.__exit__(None, None, None)
        self._stack = []

    def mm(self, out, lhsT, rhs, start=True, stop=True, reads=(), writes=()):
        return self.op("pe", lambda e: e.matmul(out, lhsT, rhs, start=start, stop=stop), reads, writes)

    def tr(self, out, in_, ident, reads=(), writes=()):
        return self.op("pe", lambda e: e.transpose(out, in_, ident), reads, writes)

    def act(self, out, in_, func, reads=(), writes=(), q="act", **kw):
        return self.op(q, lambda e: e.activation(out, in_, func, **kw), reads, writes)

    def tt(self, q, out, in0, in1, op, reads=(), writes=()):
        return self.op(q, lambda e: e.tensor_tensor(out, in0, in1, op), reads, writes)

    def ts(self, q, out, in0, s1, s2, op0, op1=None, reads=(), writes=(), **kw):
        if op1 is None:
            return self.op(q, lambda e: e.tensor_scalar(out, in0, s1, s2, op0, **kw), reads, writes)
        return self.op(q, lambda e: e.tensor_scalar(out, in0, s1, s2, op0, op1, **kw), reads, writes)

    def stt(self, q, out, in0, scalar, in1, op0, op1, reads=(), writes=()):
        return self.op(q, lambda e: e.scalar_tensor_tensor(out, in0, scalar, in1, op0, op1), reads, writes)

    def copy(self, q, out, in_, reads=(), writes=()):
        if q == "act":
            return self.op(q, lambda e: e.copy(out, in_), reads, writes)
        return self.op(q, lambda e: e.tensor_copy(out, in_), reads, writes)

    def memset(self, q, ap, val, writes=()):
        return self.op(q, lambda e: e.memset(ap, val), (), writes)


def _mk_barrier(self):
    evs = [("c", e, self.seq[e]) for e in self.COMPUTE if self.seq[e] > 0]
    for q, pool in self.dsem.items():
        for i, (sem, uses) in enumerate(pool):
            if uses > 0:
                evs.append(("d", q, i, 16 * uses))
    for q in self.ops:
        waits = self._waits_for_all(q, evs)
        if waits:
            self.ops[q].append((waits, None, None))


def _mk_waits_for_all(self, q, evs):
    need = {}
    for ev in evs:
        if ev[0] == "c":
            _, e, s = ev
            if e == q:
                continue
            key = ("c", e); sem = self.esem[e]; val = s
        else:
            _, dq, i, val = ev
            key = ("d", dq, i); sem = self.dsem[dq][i][0]
        if self.waited[q].get(key, 0) >= val:
            continue
        need[key] = (sem, val)
    out = []
    for key, (sem, val) in need.items():
        self.waited[q][key] = val
        out.append((sem, val))
    return out


def _mk_push(self):
    self._scopes = getattr(self, "_scopes", [])
    self._scopes.append(len(self._stack))


def _mk_pop(self):
    self.barrier()
    n = self._scopes.pop()
    while len(self._stack) > n:
        self._stack.pop().__exit__(None, None, None)


MK.barrier = _mk_barrier
MK._waits_for_all = _mk_waits_for_all
MK.push = _mk_push
MK.pop = _mk_pop

D = 4096
S = 4096
KC = 32
EPS = 1e-6
NEG = -1.0e6


def blkfmt(W):
    K, C = W.shape
    return np.ascontiguousarray(
        W.reshape(K // 128, 128, C // 128, 128).transpose(2, 1, 0, 3)).reshape(C // 128, 128, (K // 128) * 128)


def colfmt(v):
    return np.ascontiguousarray(v.reshape(-1, 128).T)


def rope_consts():
    inv = (np.float32(500000.0) ** (-np.arange(0, 32, 2, dtype=np.float32) / np.float32(32))).astype(np.float32)
    ang = (np.arange(S, dtype=np.float32)[:, None] * inv[None, :]).astype(np.float32)
    cos = np.cos(ang).astype(np.float32)
    sin = np.sin(ang).astype(np.float32)
    cosT = np.ones((128, S), np.float32)
    sinT = np.zeros((128, S), np.float32)
    cosT[0:16] = cos.T
    cosT[16:32] = cos.T
    sinT[0:16] = sin.T
    sinT[16:32] = sin.T
    rot = np.zeros((128, 128), np.float32)
    for mi in range(16):
        rot[mi + 16, mi] = -1.0
        rot[mi, mi + 16] = 1.0
    return cosT, sinT, rot


def rstd_inplace(m, ssq):
    m.ts("dve", ssq[:, 0:1], ssq[:, 0:1], 1.0 / D, EPS, ALU.mult, ALU.add, reads=[ssq], writes=[ssq])
    m.act(ssq[:, 0:1], ssq[:, 0:1], AF.Sqrt, reads=[ssq], writes=[ssq])
    m.op("dve", lambda e: e.reciprocal(ssq[:, 0:1], ssq[:, 0:1]), reads=[ssq], writes=[ssq])


class PsumPool:
    def __init__(self, m, n=8, prefix="P", bufs=None):
        self.bufs = bufs if bufs is not None else [m.ps(f"{prefix}{i}", [128, 512]) for i in range(n)]
        self.i = 0

    def next(self):
        b = self.bufs[self.i % len(self.bufs)]
        self.i += 1
        return b


def norm_T(m, x_rows, hT_view, Gs, SHs, st, pp, dmaq="pool"):
    xt, junk, ssq8, ssq, ident = st["xt"], st["junk"], st["ssq8"], st["ssq"], st["ident"]
    m.dma(dmaq, xt[:], x_rows, writes=[xt])
    m.memset("dve", ssq8[:], 0.0, writes=[ssq8])
    for i in range(8):
        m.act(junk[:], xt[:, i * 512:(i + 1) * 512], AF.Square, reads=[xt, ssq8], writes=[junk, ssq8],
              accum_out=ssq8[:, i:i + 1])
    m.op("dve", lambda e: e.reduce_sum(ssq[:, 0:1], ssq8[:], AX.X), reads=[ssq8], writes=[ssq])
    rstd_inplace(m, ssq)
    m.ts("dve", xt[:], xt[:], ssq[:, 0:1], None, ALU.mult, reads=[xt, ssq], writes=[xt])
    for k4 in range(8):
        P = pp.next()
        for i in range(4):
            kc = k4 * 4 + i
            m.tr(P[:, i * 128:(i + 1) * 128], xt[:, kc * 128:(kc + 1) * 128], ident[:], reads=[xt, ident], writes=[P])
        for i in range(4):
            kc = k4 * 4 + i
            out_ap, out_dep = hT_view(kc)
            m.act(out_ap, P[:, i * 128:(i + 1) * 128], AF.Identity, reads=[P, Gs, SHs], writes=[out_dep],
                  scale=Gs[:, kc:kc + 1], bias=SHs[:, kc:kc + 1])


def mod_vectors(m, modall_ap, adat_ap, ng_ap, which):
    mod = m.sb("mod", [128, 6, 32])
    tab = m.sb("tab", [128, 6, 32])
    ng = m.sb("ng", [128, 32])
    m.dma("sp", mod[:], modall_ap, writes=[mod])
    m.dma("sp", tab[:], adat_ap, writes=[tab])
    m.dma("sp", ng[:], ng_ap, writes=[ng])
    m.tt("dve", mod[:], mod[:], tab[:], ALU.add, reads=[mod, tab], writes=[mod])
    G = m.sb("Gv", [128, 32])
    SH = m.sb("SHv", [128, 32])
    GT = m.sb("GTv", [128, 32])
    o = 3 * which
    m.ts("dve", G[:], mod[:, o + 1, :], 1.0, None, ALU.add, reads=[mod], writes=[G])
    m.tt("dve", G[:], G[:], ng[:], ALU.mult, reads=[G, ng], writes=[G])
    m.copy("dve", SH[:], mod[:, o + 0, :], reads=[mod], writes=[SH])
    m.copy("dve", GT[:], mod[:, o + 2, :], reads=[mod], writes=[GT])
    return G, SH, GT


def build_L0():
    nc = bass.Bass("TRN2", target_bir_lowering=False)
    cT = nc.dram_tensor("cT", [128, 32, 4], F32, kind="ExternalInput").ap()
    w = nc.dram_tensor("w", [24, 128, 4096], F32, kind="ExternalInput").ap()
    bvec = nc.dram_tensor("b", [1, 3072], F32, kind="ExternalInput").ap()
    out = nc.dram_tensor("out", [4, 3072], F32, kind="ExternalOutput").ap()
    m = MK(nc)
    c_sb = m.sb("c_sb", [128, 32, 4])
    b_sb = m.sb("b_sb", [1, 3072])
    ones = m.sb("ones", [1, 4])
    o_sb = m.sb("o_sb", [4, 3072])
    WB = [m.sb(f"WB{i}", [128, 4096]) for i in range(2)]
    pp = PsumPool(m, 4)
    m.dma("sp", c_sb[:], cT, writes=[c_sb])
    m.dma("sp", b_sb[:], bvec, writes=[b_sb])
    m.memset("dve", ones[:], 1.0, writes=[ones])
    m.act(c_sb[:], c_sb[:], AF.Silu, reads=[c_sb], writes=[c_sb])
    for cb in range(24):
        W = WB[cb % 2]
        m.dma("sp" if cb % 2 == 0 else "pool", W[:], w[cb], writes=[W])
        P = pp.next()
        for kc in range(32):
            m.mm(P[0:4, 0:128], c_sb[:, kc, :], W[:, kc * 128:(kc + 1) * 128], start=(kc == 0), stop=False,
                 reads=[c_sb, W], writes=[P])
        m.mm(P[0:4, 0:128], ones[:], b_sb[:, cb * 128:(cb + 1) * 128], start=False, stop=True, reads=[ones, b_sb], writes=[P])
        m.copy("dve", o_sb[:, cb * 128:(cb + 1) * 128], P[0:4, 0:128], reads=[P], writes=[o_sb])
    OD = Dep("out")
    m.dma("sp", out, o_sb[:], reads=[o_sb], writes=[OD])
    m.finish([OD])
    return nc


def run_L0(inp):
    c = inp["c"]
    cT = np.ascontiguousarray(c.reshape(4, 32, 128).transpose(2, 1, 0))
    nc = build_L0()
    maps = []
    for i in range(8):
        cols = slice(3072 * i, 3072 * (i + 1))
        maps.append({"cT": cT, "w": blkfmt(inp["ada_w"][:, cols]), "b": np.ascontiguousarray(inp["ada_b"][None, cols])})
    res = run_bass_kernel_spmd(nc, maps, core_ids=list(range(8)))
    return np.concatenate([r["out"] for r in res.results], axis=1)


ZC_CB, ZC_CC, ZC_CH, ZC_Q = 0, 512, 1024, 1536
ZC_KC, ZC_VC, ZC_KS, ZC_VS, ZC_KW, ZC_VW = 2560, 2816, 3072, 3328, 3584, 3840
ZC_G = 4096
ZC_R, ZC_K, ZC_V, ZC_LWA, ZC_LG = 4224, 4736, 5248, 5760, 5888
ZROWS = 6144
ROPE_BLOCKS = set(list(range(12, 20)) + [20, 21, 24, 25, 28, 29])
TTA = 512


def a_cols(j):
    cols = []
    cw = 1024
    for part in range(3):
        cols += list(range(part * cw + 512 * j, part * cw + 512 * j + 512))
    q0 = 3 * cw
    cols += list(range(q0 + 1024 * j, q0 + 1024 * j + 1024))
    kv0 = q0 + 2048
    for slot in range(6):
        base = kv0 + slot * 512 + 256 * j
        cols += list(range(base, base + 256))
    g0 = kv0 + 3072
    cols += list(range(g0 + 24 * j, g0 + 24 * j + 24)) + [-1] * 104
    r0 = g0 + 48
    for part in range(3):
        cols += list(range(r0 + part * 1024 + 512 * j, r0 + part * 1024 + 512 * j + 512))
    cols += list(range(r0 + 3072, r0 + 3072 + 128))
    cols += list(range(r0 + 3200, r0 + 3360)) + [-1] * 96
    assert len(cols) == ZROWS
    return np.array(cols)


def phase_gemm_A(m, io, st, pp):
    x, wblk, zT = io["x"], io["wblk"], io["zT"]
    G, SH = st["G"], st["SH"]
    m.push()
    hT = m.sb("hT", [128, KC, TTA])
    WB = [m.sb(f"WB{i}", [128, 4096]) for i in range(2)]
    cs = m.sb("cs", [128, TTA])
    sn = m.sb("sn", [128, TTA])
    rot = m.sb("rot", [128, 128])
    zs = [m.sb(f"zs{i}", [128, TTA]) for i in range(2)]
    zr = [m.sb(f"zr{i}", [128, TTA]) for i in range(2)]
    m.dma("sp", rot[:], io["rot"], writes=[rot])
    wi = 0
    for tt in range(S // TTA):
        t0 = tt * TTA
        for s in range(TTA // 128):
            norm_T(m, x[t0 + s * 128:t0 + (s + 1) * 128, :],
                   lambda kc, s=s: (hT[:, kc, s * 128:(s + 1) * 128], hT), G, SH, st, pp)
        m.dma("pool", cs[:], io["cosT"][:, t0:t0 + TTA], writes=[cs])
        m.dma("pool", sn[:], io["sinT"][:, t0:t0 + TTA], writes=[sn])
        for cb in range(ZROWS // 128):
            W = WB[wi % 2]
            m.dma("sp", W[:], wblk[cb], writes=[W])
            wi += 1
            P = pp.next()
            for kc in range(KC):
                m.mm(P[:, 0:TTA], W[:, kc * 128:(kc + 1) * 128], hT[:, kc, :], start=(kc == 0), stop=(kc == KC - 1),
                     reads=[W, hT], writes=[P])
            z = zs[cb % 2]
            m.copy("act", z[:], P[:, 0:TTA], reads=[P], writes=[z])
            if cb in ROPE_BLOCKS:
                P2 = pp.next()
                m.mm(P2[:, 0:TTA], rot[:], z[:], reads=[rot, z], writes=[P2])
                r = zr[cb % 2]
                m.tt("dve", r[:], P2[:, 0:TTA], sn[:], ALU.mult, reads=[P2, sn], writes=[r])
                m.tt("dve", z[:], z[:], cs[:], ALU.mult, reads=[z, cs], writes=[z])
                m.tt("dve", z[:], z[:], r[:], ALU.add, reads=[z, r], writes=[z])
            m.dma("act", zT[cb * 128:(cb + 1) * 128, t0:t0 + TTA], z[:], reads=[z], writes=[io["zT_dep"]])
    m.pop()


def phase_conv(m, io, st):
    zT, yaT = io["zT"], io["yaT"]
    m.push()
    cw = m.sb("cw", [128, 4, 3])
    m.dma("sp", cw[:], io["convw"], writes=[cw])
    cb_t = m.sb("cvb", [128, S])
    cc_t = m.sb("cvc", [128, S])
    ch_t = m.sb("cvh", [128, S])
    zd = io["zT_dep"]
    for blk in range(4):
        m.dma("sp", cb_t[:], zT[ZC_CB + blk * 128:ZC_CB + (blk + 1) * 128, :], reads=[zd], writes=[cb_t])
        m.dma("pool", cc_t[:], zT[ZC_CC + blk * 128:ZC_CC + (blk + 1) * 128, :], reads=[zd], writes=[cc_t])
        m.dma("sp", ch_t[:], zT[ZC_CH + blk * 128:ZC_CH + (blk + 1) * 128, :], reads=[zd], writes=[ch_t])
        m.tt("dve", cc_t[:], cc_t[:], ch_t[:], ALU.mult, reads=[cc_t, ch_t], writes=[cc_t])
        m.ts("dve", ch_t[:], cc_t[:], cw[:, blk, 2:3], None, ALU.mult, reads=[cc_t, cw], writes=[ch_t])
        m.stt("dve", ch_t[:, 1:S], cc_t[:, 0:S - 1], cw[:, blk, 1:2], ch_t[:, 1:S], ALU.mult, ALU.add,
              reads=[cc_t, cw, ch_t], writes=[ch_t])
        m.stt("dve", ch_t[:, 2:S], cc_t[:, 0:S - 2], cw[:, blk, 0:1], ch_t[:, 2:S], ALU.mult, ALU.add,
              reads=[cc_t, cw, ch_t], writes=[ch_t])
        m.tt("dve", ch_t[:], ch_t[:], cb_t[:], ALU.mult, reads=[ch_t, cb_t], writes=[ch_t])
        m.dma("sp", yaT[blk * 128:(blk + 1) * 128, :], ch_t[:], reads=[ch_t], writes=[io["ya_dep"]])
    m.pop()


def build_A(phases=("gemm", "conv", "nsa", "rwkv"), dbg=False, zin=False):
    nc = bass.Bass("TRN2", target_bir_lowering=False)
    def din(name, shape):
        return nc.dram_tensor(name, list(shape), F32, kind="ExternalInput").ap()
    io = {}
    io["x"] = din("x", [S, D])
    io["modall"] = din("modall", [128, 6, 32])
    io["adat"] = din("adat", [128, 6, 32])
    io["ng"] = din("ng", [128, 32])
    io["wblk"] = din("wblk", [ZROWS // 128, 128, 4096])
    io["ident"] = din("ident", [128, 128])
    io["rot"] = din("rot", [128, 128])
    io["cosT"] = din("cosT", [128, S])
    io["sinT"] = din("sinT", [128, S])
    io["convw"] = din("convw", [128, 4, 3])
    zkind = "ExternalInput" if zin else ("ExternalOutput" if dbg else "Internal")
    io["zT"] = nc.dram_tensor("zT", [ZROWS, S], F32, kind=zkind).ap()
    io["zT_dep"] = Dep("zT")
    io["yaT"] = nc.dram_tensor("yaT", [512, S], F32, kind="ExternalOutput").ap()
    io["ya_dep"] = Dep("yaT")
    dbgd = None
    if "nsa" in phases:
        nsa_decl(nc, io, din)
        if dbg:
            dbgd = {"kcmpT": nc.dram_tensor("d_kcmpT", [128, 256], F32, kind="ExternalOutput").ap(),
                    "vaug": nc.dram_tensor("d_vaug", [256, 193], F32, kind="ExternalOutput").ap(),
                    "biasT": nc.dram_tensor("d_biasT", [64, S], F32, kind="ExternalOutput").ap(),
                    "dep": Dep("dbg")}
    if "rwkv" in phases:
        rwkv_decl(nc, io, din)
    m = MK(nc)
    banks = [m.ps(f"B{i}", [128, 512]) for i in range(8)]
    pp = PsumPool(m, bufs=banks[0:5])
    po = PsumPool(m, bufs=banks[5:7])
    st = {"banks": banks}
    st["ident"] = m.sb("ident", [128, 128])
    m.dma("sp", st["ident"][:], io["ident"], writes=[st["ident"]])
    finals = []
    if "gemm" in phases:
        m.push()
        st["xt"] = m.sb("xt", [128, D])
        st["junk"] = m.sb("junk", [128, 512])
        st["ssq8"] = m.sb("ssq8", [128, 8])
        st["ssq"] = m.sb("ssq", [128, 1])
        st["G"], st["SH"], st["GT"] = mod_vectors(m, io["modall"], io["adat"], io["ng"], 0)
        phase_gemm_A(m, io, st, pp)
        m.pop()
        if dbg:
            finals.append(io["zT_dep"])
    if "conv" in phases:
        phase_conv(m, io, st)
        finals.append(io["ya_dep"])
    if "nsa" in phases:
        phase_nsa(m, io, st, pp, po, dbg=dbgd)
        finals.append(io["yb_dep"])
        if dbgd:
            finals.append(dbgd["dep"])
    if "rwkv" in phases:
        phase_rwkv(m, io, st, pp, po)
        finals.append(io["yc_dep"])
    m.finish(finals)
    return nc, m


def modfmt(v6):
    return np.ascontiguousarray(v6.reshape(6, 32, 128).transpose(2, 0, 1))


def weights_A(inp, l, j):
    cols = a_cols(j)
    w = inp["w_in"][l]
    wc = np.zeros((D, ZROWS), np.float32)
    valid = cols >= 0
    wc[:, valid] = w[:, cols[valid]]
    return {"wblk": blkfmt(wc),
            "convw": np.ascontiguousarray(inp["conv_w"][l][:, 512 * j:512 * j + 512].reshape(3, 4, 128).transpose(2, 1, 0))}


def inputs_A(inp, l, x_cur, mod_all, b, j, consts, wA):
    d = {
        "x": np.ascontiguousarray(x_cur[b]),
        "modall": modfmt(mod_all[b].reshape(6, D)),
        "adat": modfmt(inp["ada_table"][l]),
        "ng": colfmt(inp["norm_g"][l, 0]),
        "ident": consts["ident"], "rot": consts["rot"], "cosT": consts["cosT"], "sinT": consts["sinT"],
    }
    d.update(wA)
    return d

SCALE = float(128 ** -0.5)


def nsa_consts():
    E = np.zeros((64, 32, 128), np.float32)
    for kt in range(32):
        E[2 * kt, kt, 0:64] = 1.0
        E[2 * kt + 1, kt, 64:128] = 1.0
    kk = np.arange(128)[:, None]
    qq = np.arange(128)[None, :]
    tri = np.where(kk <= qq, 0.0, NEG).astype(np.float32)
    atri = np.where(kk > qq, 0.0, NEG).astype(np.float32)
    n = np.arange(256)[:, None]
    t = np.arange(S)[None, :]
    cbias = np.where((16 * n + 31 <= t) & (n < 255), 0.0, NEG).astype(np.float32)
    tt = np.arange(S)[:, None]
    j = np.arange(64)[None, :]
    cur = tt // 64
    forced = np.zeros((S, 64), np.float32)
    forced = np.maximum(forced, np.where(j == cur - 1, 1e9, 0.0))
    forced = np.maximum(forced, np.where(j == cur, 2e9, 0.0))
    forced = np.maximum(forced, np.where(j == 0, 3e9, 0.0)).astype(np.float32)
    allow = np.where(j <= cur, 4e9, -1e30).astype(np.float32)
    i = np.arange(255)[:, None] * 16
    jb = np.arange(64)[None, :] * 64
    inter = np.clip(np.minimum(i + 32, jb + 64) - np.maximum(i, jb), 0, None)
    bmap = np.zeros((256, 64), np.float32)
    bmap[:255] = inter.astype(np.float32) / np.float32(32)
    return {"E": E.reshape(64, 4096), "tri": tri, "atri": atri, "cbias": cbias, "forced": forced, "allow": allow,
            "bmap": bmap}


def nsa_decl(nc, io, din):
    io["cw1"] = din("cw1", [2, 128, 4096])
    io["cw2"] = din("cw2", [2, 128, 128])
    io["cpeT"] = din("cpeT", [2, 128, 32])
    io["E"] = din("E", [64, 4096])
    io["tri"] = din("tri", [128, 128])
    io["atri"] = din("atri", [128, 128])
    io["cbias"] = din("cbias", [256, S])
    io["forced"] = din("forced", [S, 64])
    io["allow"] = din("allow", [S, 64])
    io["bmap"] = din("bmap", [256, 64])
    io["yb"] = nc.dram_tensor("yb", [S, 1024], F32, kind="ExternalOutput").ap()
    io["yb_dep"] = Dep("yb")


def nsa_inputs(inp, l, consts):
    d = {k: consts[k] for k in ("E", "tri", "atri", "cbias", "forced", "allow", "bmap")}
    d["cw1"] = np.ascontiguousarray(inp["cmp_w1"][l].reshape(2, 32, 128, 128).transpose(0, 2, 1, 3)).reshape(2, 128, 4096)
    d["cw2"] = np.ascontiguousarray(inp["cmp_w2"][l])
    d["cpeT"] = np.ascontiguousarray(inp["cmp_pe"][l].transpose(0, 2, 1))
    return d


def phase_nsa(m, io, st, pp, po, dbg=None):
    zT = io["zT"]
    zd = io["zT_dep"]
    ident = st["ident"]
    m.push()
    E = m.sb("E", [64, 4096])
    tri = m.sb("tri", [128, 128])
    atri = m.sb("atri", [128, 128])
    m.dma("sp", E[:], io["E"], writes=[E])
    m.dma("sp", tri[:], io["tri"], writes=[tri])
    m.dma("sp", atri[:], io["atri"], writes=[atri])
    GTK = m.sb("GTK", [128, 32, 24])
    m.push()
    gT = m.sb("gT", [24, S])
    m.dma("sp", gT[:], zT[ZC_G:ZC_G + 24, :], reads=[zd], writes=[gT])
    for Q in range(32):
        P = pp.next()
        m.tr(P[:, 0:24], gT[:, Q * 128:(Q + 1) * 128], ident[0:24, 0:24], reads=[gT, ident], writes=[P])
        m.act(GTK[:, Q, :], P[:, 0:24], AF.Sigmoid, reads=[P], writes=[GTK])
    m.pop()

    KCMP = m.sb("KCMP", [128, 256])
    VAUG = m.sb("VAUG", [128, 2, 193])
    ksT = m.sb("ksT", [128, S])
    kwT = m.sb("kwT", [128, S])
    VS = m.sb("VS", [128, 32, 129])
    VW = m.sb("VW", [128, 32, 129])
    qT = [m.sb(f"qT{i}", [128, 4, 128]) for i in range(2)]
    cbt = [m.sb(f"cbt{i}", [128, 2, 128]) for i in range(2)]
    fct = [m.sb(f"fct{i}", [128, 64]) for i in range(2)]
    alt = [m.sb(f"alt{i}", [128, 64]) for i in range(2)]
    ec = m.sb("ec", [128, 256])
    ES = [m.sb(f"ES{i}", [128, 512]) for i in range(2)]
    OACC = m.sb("OACC", [128, 4, 128])
    IMP = m.sb("IMP", [128, 64])
    IMP2 = m.sb("IMP2", [128, 64])
    M8a = m.sb("M8a", [128, 8])
    M8b = m.sb("M8b", [128, 8])
    MSK = m.sb("MSK", [128, 64])
    biasT = m.sb("biasT", [64, 128])
    rden = m.sb("rden", [128, 1])
    otmp = m.sb("otmp", [128, 128])

    for lg in range(2):
        m.push()
        XT = m.sb("XT", [128, S])
        W1 = m.sb("W1", [128, 4096])
        W2 = m.sb("W2", [128, 128])
        PET = m.sb("PET", [128, 32])
        bcol = m.sb("bcol", [128, 1])
        xs = m.sb("xs", [128, 255])
        x2 = m.sb("x2", [128, 255])
        m.memset("dve", VAUG[:], 0.0, writes=[VAUG])
        m.memset("dve", VAUG[:, :, 128:129], 1.0, writes=[VAUG])
        for nt in range(2):
            m.dma("pool", VAUG[:, nt, 129:193], io["bmap"][nt * 128:(nt + 1) * 128, :], writes=[VAUG])
        m.memset("dve", KCMP[:], 0.0, writes=[KCMP])
        for X in range(2):
            row0 = (ZC_KC if X == 0 else ZC_VC) + lg * 128
            m.dma("sp", XT[:], zT[row0:row0 + 128, :], reads=[zd], writes=[XT])
            m.dma("sp", W1[:], io["cw1"][X], writes=[W1])
            m.dma("pool", W2[:], io["cw2"][X], writes=[W2])
            m.dma("pool", PET[:], io["cpeT"][X], writes=[PET])
            XV = XT[:].rearrange("p (n s) -> p n s", s=16)
            P = pp.next()
            for l_ in range(32):
                n0 = l_ // 16
                m.mm(P[:, 0:255], W1[:, l_ * 128:(l_ + 1) * 128], XV[:, n0:n0 + 255, l_ % 16], start=(l_ == 0), stop=(l_ == 31),
                     reads=[W1, XT], writes=[P])
            Pb = pp.next()
            for l_ in range(32):
                m.mm(Pb[:, 0:1], W1[:, l_ * 128:(l_ + 1) * 128], PET[:, l_:l_ + 1], start=(l_ == 0), stop=(l_ == 31),
                     reads=[W1, PET], writes=[Pb])
            m.copy("dve", bcol[:], Pb[:, 0:1], reads=[Pb], writes=[bcol])
            m.act(xs[:], P[:, 0:255], AF.Identity, reads=[P, bcol], writes=[xs], bias=bcol[:, 0:1], scale=1.0)
            m.tt("dve", x2[:], xs[:], xs[:], ALU.mult, reads=[xs], writes=[x2])
            m.ts("dve", x2[:], x2[:], 0.044715, 1.0, ALU.mult, ALU.add, reads=[x2], writes=[x2])
            m.tt("dve", x2[:], x2[:], xs[:], ALU.mult, reads=[x2, xs], writes=[x2])
            m.act(x2[:], x2[:], AF.Tanh, reads=[x2], writes=[x2], scale=0.7978845608028654)
            m.stt("dve", x2[:], x2[:], 1.0, xs[:], ALU.add, ALU.mult, reads=[x2, xs], writes=[x2])
            m.ts("dve", x2[:], x2[:], 0.5, None, ALU.mult, reads=[x2], writes=[x2])
            if X == 0:
                P3 = pp.next()
                m.mm(P3[:, 0:255], W2[:], x2[:], reads=[W2, x2], writes=[P3])
                m.copy("dve", KCMP[:, 0:255], P3[:, 0:255], reads=[P3], writes=[KCMP])
            else:
                for nt in range(2):
                    nn = 128 if nt == 0 else 127
                    P3 = pp.next()
                    m.mm(P3[0:nn, 0:128], x2[:, nt * 128:nt * 128 + nn], W2[:], reads=[x2, W2], writes=[P3])
                    m.copy("dve", VAUG[0:nn, nt, 0:128], P3[0:nn, 0:128], reads=[P3], writes=[VAUG])
        if dbg is not None and lg == 0:
            m.dma("sp", dbg["kcmpT"], KCMP[:], reads=[KCMP], writes=[dbg["dep"]])
            m.dma("sp", dbg["vaug"].rearrange("(nt p) c -> p nt c", p=128), VAUG[:], reads=[VAUG], writes=[dbg["dep"]])
        m.dma("sp", ksT[:], zT[ZC_KS + lg * 128:ZC_KS + (lg + 1) * 128, :], reads=[zd], writes=[ksT])
        m.dma("sp", kwT[:], zT[ZC_KW + lg * 128:ZC_KW + (lg + 1) * 128, :], reads=[zd], writes=[kwT])
        for (VV, zc) in ((VS, ZC_VS), (VW, ZC_VW)):
            m.dma("sp", XT[:], zT[zc + lg * 128:zc + (lg + 1) * 128, :], reads=[zd], writes=[XT])
            m.memset("dve", VV[:, :, 128:129], 1.0, writes=[VV])
            for k4 in range(8):
                P = pp.next()
                for i in range(4):
                    kt = k4 * 4 + i
                    m.tr(P[:, i * 128:(i + 1) * 128], XT[:, kt * 128:(kt + 1) * 128], ident[:], reads=[XT, ident], writes=[P])
                m.copy("act", VV[:, k4 * 4:k4 * 4 + 4, 0:128], P[:].rearrange("p (c t) -> p c t", t=128), reads=[P], writes=[VV])
        m.pop()
        for Q in range(32):
            qt = qT[Q % 2]
            cb_ = cbt[Q % 2]
            fc_ = fct[Q % 2]
            al_ = alt[Q % 2]
            r0 = ZC_Q + lg * 512
            m.dma("sp", qt[:], zT[r0:r0 + 512, Q * 128:(Q + 1) * 128].rearrange("(h p) t -> p h t", p=128), reads=[zd], writes=[qt])
            m.dma("pool", cb_[:], io["cbias"][:, Q * 128:(Q + 1) * 128].rearrange("(nt p) t -> p nt t", p=128), writes=[cb_])
            m.dma("pool", fc_[:], io["forced"][Q * 128:(Q + 1) * 128, :], writes=[fc_])
            m.dma("pool", al_[:], io["allow"][Q * 128:(Q + 1) * 128, :], writes=[al_])
            for h in range(4):
                gcol = (lg * 4 + h) * 3
                P = pp.next()
                for nt in range(2):
                    m.mm(P[:, nt * 128:(nt + 1) * 128], KCMP[:, nt * 128:(nt + 1) * 128], qt[:, h, :], start=True, stop=False,
                         reads=[KCMP, qt], writes=[P])
                    m.mm(P[:, nt * 128:(nt + 1) * 128], ident[:], cb_[:, nt, :], start=False, stop=True,
                         reads=[ident, cb_], writes=[P])
                m.act(ec[:], P[:, 0:256], AF.Exp, reads=[P], writes=[ec], scale=SCALE)
                P2 = pp.next()
                for nt in range(2):
                    m.mm(P2[:, 0:193], ec[:, nt * 128:(nt + 1) * 128], VAUG[:, nt, :], start=(nt == 0), stop=(nt == 1),
                         reads=[ec, VAUG], writes=[P2])
                m.ts("dve", rden[:], P2[:, 128:129], 1e-30, None, ALU.max, reads=[P2], writes=[rden])
                m.op("dve", lambda e: e.reciprocal(rden[:], rden[:]), reads=[rden], writes=[rden])
                m.ts("dve", OACC[:, h, :], P2[:, 0:128], rden[:, 0:1], GTK[:, Q, gcol:gcol + 1], ALU.mult, ALU.mult,
                     reads=[P2, rden, GTK], writes=[OACC])
                if h == 0:
                    m.ts("dve", IMP[:], P2[:, 129:193], rden[:, 0:1], None, ALU.mult, reads=[P2, rden], writes=[IMP])
                else:
                    m.stt("dve", IMP[:], P2[:, 129:193], rden[:, 0:1], IMP[:], ALU.mult, ALU.add, reads=[P2, rden, IMP], writes=[IMP])
            m.tt("dve", IMP[:], IMP[:], fc_[:], ALU.max, reads=[IMP, fc_], writes=[IMP])
            m.tt("dve", IMP[:], IMP[:], al_[:], ALU.min, reads=[IMP, al_], writes=[IMP])
            m.op("dve", lambda e: e.max(M8a[:], IMP[:]), reads=[IMP], writes=[M8a])
            m.op("dve", lambda e: e.match_replace(IMP2[:], M8a[:], IMP[:], -3.0e38), reads=[IMP, M8a], writes=[IMP2])
            m.op("dve", lambda e: e.max(M8b[:], IMP2[:]), reads=[IMP2], writes=[M8b])
            m.ts("dve", MSK[:], IMP[:], M8b[:, 7:8], None, ALU.is_ge, reads=[IMP, M8b], writes=[MSK])
            m.ts("dve", MSK[:], MSK[:], -NEG, NEG, ALU.mult, ALU.add, reads=[MSK], writes=[MSK])
            P = pp.next()
            m.tr(P[0:64, 0:128], MSK[:], ident[:], reads=[MSK, ident], writes=[P])
            m.copy("dve", biasT[:], P[0:64, 0:128], reads=[P], writes=[biasT])
            if dbg is not None and lg == 0:
                m.dma("sp", dbg["biasT"][:, Q * 128:(Q + 1) * 128], biasT[:], reads=[biasT], writes=[dbg["dep"]])
            ei = 0
            for h in range(4):
                gcol = (lg * 4 + h) * 3
                for br in range(2):
                    KT_, VV = (ksT, VS) if br == 0 else (kwT, VW)
                    kts = list(range(0, Q + 1)) if br == 0 else list(range(max(0, Q - 4), Q + 1))
                    PO = po.next()
                    for g0 in range(0, len(kts), 4):
                        grp = kts[g0:g0 + 4]
                        P = pp.next()
                        for i, kt in enumerate(grp):
                            extra = []
                            if br == 0:
                                extra.append((E[:, kt * 128:(kt + 1) * 128], biasT[:], [E, biasT]))
                            if kt == Q:
                                extra.append((ident[:], tri[:], [ident, tri]))
                            if br == 1 and kt == Q - 4:
                                extra.append((ident[:], atri[:], [ident, atri]))
                            m.mm(P[:, i * 128:(i + 1) * 128], KT_[:, kt * 128:(kt + 1) * 128], qt[:, h, :], start=True,
                                 stop=(len(extra) == 0), reads=[KT_, qt], writes=[P])
                            for xi, (lh_, rh_, rd_) in enumerate(extra):
                                m.mm(P[:, i * 128:(i + 1) * 128], lh_, rh_, start=False, stop=(xi == len(extra) - 1),
                                     reads=rd_, writes=[P])
                        es = ES[ei % 2]
                        ei += 1
                        w = len(grp) * 128
                        m.act(es[:, 0:w], P[:, 0:w], AF.Exp, reads=[P], writes=[es], scale=SCALE)
                        for i, kt in enumerate(grp):
                            m.mm(PO[:, 0:129], es[:, i * 128:(i + 1) * 128], VV[:, kt, :], start=(kt == kts[0]), stop=(kt == kts[-1]),
                                 reads=[es, VV], writes=[PO])
                    m.op("dve", lambda e, PO=PO: e.reciprocal(rden[:], PO[:, 128:129]), reads=[PO], writes=[rden])
                    m.ts("dve", otmp[:], PO[:, 0:128], rden[:, 0:1], GTK[:, Q, gcol + 1 + br:gcol + 2 + br], ALU.mult, ALU.mult,
                         reads=[PO, rden, GTK], writes=[otmp])
                    m.tt("dve", OACC[:, h, :], OACC[:, h, :], otmp[:], ALU.add, reads=[OACC, otmp], writes=[OACC])
            m.dma("act", io["yb"][Q * 128:(Q + 1) * 128, lg * 512:(lg + 1) * 512], OACC[:].rearrange("p h d -> p (h d)"),
                  reads=[OACC], writes=[io["yb_dep"]])
    m.pop()

RCH = 256
RTC = 16
LN_EPS = 64e-5


def rwkv_decl(nc, io, din):
    for nm in ("mu_r", "mu_k", "mu_v", "w0", "a0", "kks", "ka", "lnw", "lnb", "rk"):
        io["r_" + nm] = din("r_" + nm, [128, 4])
    io["r_mu_lwa"] = din("r_mu_lwa", [128, 1])
    io["r_mu_lg"] = din("r_mu_lg", [128, 2])
    io["r_wbp"] = din("r_wbp", [128, 512])
    io["r_abp"] = din("r_abp", [128, 512])
    io["r_gb1"] = din("r_gb1", [128, 512])
    io["r_gb2"] = din("r_gb2", [32, 512])
    io["r_bones"] = din("r_bones", [128, 128])
    io["ycT"] = nc.dram_tensor("ycT", [512, S], F32, kind="ExternalOutput").ap()
    io["yc_dep"] = Dep("ycT")
    io["TM"] = nc.dram_tensor("r_TM", [S, 4, 4, 128], F32, kind="Internal").ap()
    io["TV"] = nc.dram_tensor("r_TV", [S, 512], F32, kind="Internal").ap()
    io["TO"] = nc.dram_tensor("r_TO", [S, 512], F32, kind="Internal").ap()


def rwkv_inputs(inp, l, j):
    sl = slice(512 * j, 512 * j + 512)
    mu = inp["rwkv_mu"][l]
    d = {}
    d["r_mu_r"] = colfmt(mu[0:1024][sl])
    d["r_mu_k"] = colfmt(mu[1024:2048][sl])
    d["r_mu_v"] = colfmt(mu[2048:3072][sl])
    d["r_mu_lwa"] = np.ascontiguousarray(mu[3072:3200][:, None])
    lgm = np.zeros(256, np.float32)
    lgm[:160] = mu[3200:3360]
    d["r_mu_lg"] = colfmt(lgm)
    d["r_w0"] = colfmt(inp["rwkv_w0"][l][sl])
    d["r_a0"] = colfmt(inp["rwkv_a0"][l][sl])
    d["r_kks"] = colfmt(inp["rwkv_kk"][l][sl])
    d["r_ka"] = colfmt(inp["rwkv_ka"][l][sl])
    d["r_lnw"] = colfmt(inp["rwkv_ln_w"][l][sl])
    d["r_lnb"] = colfmt(inp["rwkv_ln_b"][l][sl])
    d["r_rk"] = colfmt(inp["rwkv_rk"][l].reshape(-1)[sl])
    wbp = np.zeros((128, 512), np.float32)
    wbp[:64] = inp["rwkv_wb"][l][:, sl]
    abp = np.zeros((128, 512), np.float32)
    abp[64:] = inp["rwkv_ab"][l][:, sl]
    d["r_wbp"] = wbp
    d["r_abp"] = abp
    d["r_gb1"] = np.ascontiguousarray(inp["rwkv_gb"][l][:128, sl])
    d["r_gb2"] = np.ascontiguousarray(inp["rwkv_gb"][l][128:160, sl])
    bones = np.zeros((128, 128), np.float32)
    bones[:64, :64] = 1.0
    bones[64:, 64:] = 1.0
    d["r_bones"] = bones
    return d


def phase_rwkv(m, io, st, pp, po):
    zT, zd = io["zT"], io["zT_dep"]
    ident = st["ident"]
    TM, TV, TO = io["TM"], io["TV"], io["TO"]
    TMd, TVd, TOd = Dep("TM"), Dep("TV"), Dep("TO")
    m.push()
    C = {}
    for nm in ("mu_r", "mu_k", "mu_v", "w0", "a0", "kks", "ka", "lnw", "lnb", "rk"):
        C[nm] = m.sb("c_" + nm, [128, 4])
        m.dma("sp", C[nm][:], io["r_" + nm], writes=[C[nm]])
    mu_lwa = m.sb("c_mu_lwa", [128, 1]); m.dma("sp", mu_lwa[:], io["r_mu_lwa"], writes=[mu_lwa])
    mu_lg = m.sb("c_mu_lg", [128, 2]); m.dma("sp", mu_lg[:], io["r_mu_lg"], writes=[mu_lg])
    WBP = m.sb("WBP", [128, 512]); m.dma("sp", WBP[:], io["r_wbp"], writes=[WBP])
    ABP = m.sb("ABP", [128, 512]); m.dma("sp", ABP[:], io["r_abp"], writes=[ABP])
    GB1 = m.sb("GB1", [128, 512]); m.dma("sp", GB1[:], io["r_gb1"], writes=[GB1])
    GB2 = m.sb("GB2", [32, 512]); m.dma("sp", GB2[:], io["r_gb2"], writes=[GB2])
    BONES = m.sb("BONES", [128, 128]); m.dma("sp", BONES[:], io["r_bones"], writes=[BONES])
    OM = {}
    for nm in ("mu_r", "mu_k", "mu_v"):
        OM[nm] = m.sb("om_" + nm, [128, 4])
        m.ts("dve", OM[nm][:], C[nm][:], -1.0, 1.0, ALU.mult, ALU.add, reads=[C[nm]], writes=[OM[nm]])
    om_lwa = m.sb("om_lwa", [128, 1])
    m.ts("dve", om_lwa[:], mu_lwa[:], -1.0, 1.0, ALU.mult, ALU.add, reads=[mu_lwa], writes=[om_lwa])
    om_lg = m.sb("om_lg", [128, 2])
    m.ts("dve", om_lg[:], mu_lg[:], -1.0, 1.0, ALU.mult, ALU.add, reads=[mu_lg], writes=[om_lg])
    W0N = m.sb("W0N", [128, 4])
    m.ts("dve", W0N[:], C["w0"][:], -1.0, None, ALU.mult, reads=[C["w0"]], writes=[W0N])
    OMKA = m.sb("OMKA", [128, 4])
    m.ts("dve", OMKA[:], C["ka"][:], -1.0, 1.0, ALU.mult, ALU.add, reads=[C["ka"]], writes=[OMKA])

    W = RCH + 1
    ust = [m.sb(f"ust{i}", [128, W]) for i in range(3)]
    utmp = m.sb("utmp", [128, RCH])
    TLA = m.sb("TLA", [128, RCH])
    SG1 = m.sb("SG1", [128, RCH])
    SG2 = m.sb("SG2", [32, RCH])
    rm = m.sb("rm", [128, RCH]); km = m.sb("km", [128, RCH]); vm = m.sb("vm", [128, RCH])
    kkn = m.sb("kkn", [128, W])
    t1 = m.sb("t1", [128, RCH]); t2 = m.sb("t2", [128, RCH]); At = m.sb("At", [128, RCH])
    kp = m.sb("kp", [128, RCH]); nka = m.sb("nka", [128, RCH])
    WT = [m.sb(f"WT{g}", [128, RCH]) for g in range(4)]
    BON = [m.sb(f"BON{g}", [128, RCH]) for g in range(4)]
    GTt = [m.sb(f"GTt{g}", [128, RCH]) for g in range(4)]
    KKR = [m.sb(f"KKR{g}", [128, RCH, 4]) for g in range(4)]
    KK0 = [m.sb(f"KK0{g}", [128, 4]) for g in range(4)]
    TMs = m.sb("TMs", [128, 4, 128])
    TVs = m.sb("TVs", [128, 128])
    LL = [[m.sb(f"LL{g}_{b}", [6, RTC, 128]) for b in range(2)] for g in range(4)]
    OSV = [[m.sb(f"OSV{g}_{b}", [6, RTC + 1, 64]) for b in range(2)] for g in range(4)]
    ST = [m.sb(f"ST{g}", [128, 64]) for g in range(4)]
    banks = st["banks"]
    pp = PsumPool(m, bufs=banks[0:2])
    UB = banks[2:6]
    OB = banks[6:8]
    ot_s = m.sb("ot_s", [128, 128])
    OT = m.sb("OT", [128, RCH]); cen = m.sb("cen", [128, RCH]); sq = m.sb("sq", [128, RCH])

    for g in range(4):
        m.memset("dve", ST[g][:], 0.0, writes=[ST[g]])
        m.memset("pool", KKR[g][:], 0.0, writes=[KKR[g]])
        m.memset("pool", KK0[g][:], 0.0, writes=[KK0[g]])
        for b in range(2):
            m.memset("pool", LL[g][b][:], 0.0, writes=[LL[g][b].d("z"), LL[g][b].d("l")])
            m.memset("pool", OSV[g][b][:], 0.0, writes=[OSV[g][b].d("os"), OSV[g][b].d("v")])
    m.memset("dve", TMs[:], 0.0, writes=[TMs])

    def load_shift_mix(row0, nrows, mu_col, om_col, out_t, si):
        u = ust[si]
        if t0 == 0:
            m.memset("dve", u[0:nrows, 0:1], 0.0, writes=[u])
            m.dma("sp", u[0:nrows, 1:W], zT[row0:row0 + nrows, 0:RCH], reads=[zd], writes=[u])
        else:
            m.dma("sp", u[0:nrows, :], zT[row0:row0 + nrows, t0 - 1:t0 + RCH], reads=[zd], writes=[u])
        m.ts("dve", utmp[0:nrows, :], u[0:nrows, 0:RCH], mu_col, None, ALU.mult, reads=[u], writes=[utmp])
        m.stt("dve", out_t[0:nrows, :], u[0:nrows, 1:W], om_col, utmp[0:nrows, :], ALU.mult, ALU.add, reads=[u, utmp], writes=[out_t])

    nchunks = globals().get("RW_NCH", S // RCH)
    for c in range(nchunks):
        t0 = c * RCH
        load_shift_mix(ZC_LWA, 128, mu_lwa[:, 0:1], om_lwa[:, 0:1], TLA, 0)
        m.act(TLA[0:64, :], TLA[0:64, :], AF.Tanh, reads=[TLA], writes=[TLA])
        load_shift_mix(ZC_LG, 128, mu_lg[:, 0:1], om_lg[:, 0:1], SG1, 1)
        m.act(SG1[:], SG1[:], AF.Sigmoid, reads=[SG1], writes=[SG1])
        load_shift_mix(ZC_LG + 128, 32, mu_lg[0:32, 1:2], om_lg[0:32, 1:2], SG2, 2)
        m.act(SG2[:], SG2[:], AF.Sigmoid, reads=[SG2], writes=[SG2])
        for g in range(4):
            gs_ = slice(g * 128, (g + 1) * 128)
            load_shift_mix(ZC_R + g * 128, 128, C["mu_r"][:, g:g + 1], OM["mu_r"][:, g:g + 1], rm, 0)
            load_shift_mix(ZC_K + g * 128, 128, C["mu_k"][:, g:g + 1], OM["mu_k"][:, g:g + 1], km, 1)
            load_shift_mix(ZC_V + g * 128, 128, C["mu_v"][:, g:g + 1], OM["mu_v"][:, g:g + 1], vm, 2)
            P = pp.next()
            m.mm(P[:, 0:RCH], WBP[:, gs_], TLA[:], reads=[WBP, TLA], writes=[P])
            m.act(t1[:], P[:, 0:RCH], AF.Exp, reads=[P, W0N], writes=[t1], scale=-1.0, bias=W0N[:, g:g + 1])
            m.act(t1[:], t1[:], AF.Ln, reads=[t1], writes=[t1], bias=1.0, scale=1.0)
            m.act(t1[:], t1[:], AF.Exp, reads=[t1], writes=[t1], scale=-1.0, bias=-0.5)
            m.act(WT[g][:], t1[:], AF.Exp, reads=[t1], writes=[WT[g]], scale=-1.0)
            P = pp.next()
            m.mm(P[:, 0:RCH], ABP[:, gs_], TLA[:], reads=[ABP, TLA], writes=[P])
            m.act(At[:], P[:, 0:RCH], AF.Sigmoid, reads=[P, C["a0"]], writes=[At], bias=C["a0"][:, g:g + 1], scale=1.0)
            P = pp.next()
            m.mm(P[:, 0:RCH], GB1[:, gs_], SG1[:], start=True, stop=False, reads=[GB1, SG1], writes=[P])
            m.mm(P[:, 0:RCH], GB2[:, gs_], SG2[:], start=False, stop=True, reads=[GB2, SG2], writes=[P])
            m.copy("act", GTt[g][:], P[:, 0:RCH], reads=[P], writes=[GTt[g]])
            m.ts("dve", t1[:], km[:], C["kks"][:, g:g + 1], None, ALU.mult, reads=[km, C["kks"]], writes=[t1])
            m.tt("dve", t2[:], t1[:], t1[:], ALU.mult, reads=[t1], writes=[t2])
            P = pp.next()
            m.mm(P[:, 0:RCH], BONES[:], t2[:], reads=[BONES, t2], writes=[P])
            m.act(t2[:], P[:, 0:RCH], AF.Sqrt, reads=[P], writes=[t2])
            m.ts("dve", t2[:], t2[:], 1e-12, None, ALU.max, reads=[t2], writes=[t2])
            m.op("dve", lambda e: e.reciprocal(t2[:], t2[:]), reads=[t2], writes=[t2])
            m.tt("dve", kkn[:, 1:W], t1[:], t2[:], ALU.mult, reads=[t1, t2], writes=[kkn])
            m.ts("dve", t1[:], At[:], C["ka"][:, g:g + 1], OMKA[:, g:g + 1], ALU.mult, ALU.add, reads=[At, C["ka"], OMKA], writes=[t1])
            m.tt("dve", kp[:], km[:], t1[:], ALU.mult, reads=[km, t1], writes=[kp])
            m.stt("dve", nka[:], kkn[:, 1:W], -1.0, At[:], ALU.mult, ALU.mult, reads=[kkn, At], writes=[nka])
            m.tt("dve", t1[:], rm[:], kp[:], ALU.mult, reads=[rm, kp], writes=[t1])
            m.ts("dve", t1[:], t1[:], C["rk"][:, g:g + 1], None, ALU.mult, reads=[t1, C["rk"]], writes=[t1])
            P = pp.next()
            m.mm(P[:, 0:RCH], BONES[:], t1[:], reads=[BONES, t1], writes=[P])
            m.tt("dve", BON[g][:], P[:, 0:RCH], vm[:], ALU.mult, reads=[P, vm], writes=[BON[g]])
            K4 = KKR[g]
            m.copy("pool", K4[0:64, :, 0], rm[0:64, :], reads=[rm], writes=[K4])
            m.copy("pool", K4[64:128, :, 1], rm[64:128, :], reads=[rm], writes=[K4])
            m.copy("pool", K4[0:64, 0:RCH - 1, 2], kkn[0:64, 2:W], reads=[kkn], writes=[K4])
            m.copy("pool", K4[64:128, 0:RCH - 1, 3], kkn[64:128, 2:W], reads=[kkn], writes=[K4])
            m.copy("pool", KK0[g][0:64, 2:3], kkn[0:64, 1:2], reads=[kkn], writes=[KK0[g]])
            m.copy("pool", KK0[g][64:128, 3:4], kkn[64:128, 1:2], reads=[kkn], writes=[KK0[g]])
            for s in range(RCH // 128):
                P = pp.next()
                m.tr(P[:, 0:128], nka[:, s * 128:(s + 1) * 128], ident[:], reads=[nka, ident], writes=[P])
                m.tr(P[:, 128:256], kp[:, s * 128:(s + 1) * 128], ident[:], reads=[kp, ident], writes=[P])
                m.tr(P[:, 256:384], vm[:, s * 128:(s + 1) * 128], ident[:], reads=[vm, ident], writes=[P])
                Pv = P[:, 0:256].rearrange("p (w h k) -> p w h k", w=2, h=2)
                for h in range(2):
                    m.copy("act", TMs[:, h:4:2, h * 64:(h + 1) * 64], Pv[:, :, h, :], reads=[P], writes=[TMs])
                m.copy("act", TVs[:], P[:, 256:384], reads=[P], writes=[TVs])
                tsl = slice(t0 + s * 128, t0 + (s + 1) * 128)
                m.dma("act", TM[tsl, g, :, :], TMs[:], reads=[TMs], writes=[TMd])
                m.dma("act", TV[tsl, gs_], TVs[:], reads=[TVs], writes=[TVd])
        for sc in range(RCH // RTC if globals().get("RW_STAGE", 3) >= 2 else 0):
            b = sc % 2
            ts0 = t0 + sc * RTC
            for g in range(4):
                L = LL[g][b]
                U = OSV[g][b]
                m.dma("sp", L[2:6, :, :], TM[ts0:ts0 + RTC, g, :, :].rearrange("t r c -> r t c"), reads=[TMd], writes=[L.d("l")])
                m.dma("pool", U[4:6, 0:RTC, :], TV[ts0:ts0 + RTC, g * 128:(g + 1) * 128].rearrange("t (h k) -> h t k", h=2),
                      reads=[TVd], writes=[U.d("v")])
            for tl in range(RTC):
                tc = sc * RTC + tl
                for g in range(4):
                    L = LL[g][b]
                    U = OSV[g][b]
                    pu = UB[g][:, 0:64]
                    pd = UB[g]
                    pso = OB[g // 2][0:4, (g % 2) * 64:(g % 2) * 64 + 64]
                    psd = OB[g // 2]
                    if tc == 0:
                        m.mm(pso, KK0[g][:], ST[g][:], reads=[KK0[g], ST[g]], writes=[psd])
                        m.copy("act", U[0:4, 0, :], pso, reads=[psd], writes=[U.d("os")])
                    m.mm(pu, L[:, tl, :], U[:, tl, :], reads=[L.d("l"), L.d("z"), U.d("os"), U.d("v")], writes=[pd])
                    m.stt("dve", ST[g][:], ST[g][:], WT[g][:, tc:tc + 1], pu, ALU.mult, ALU.add, reads=[ST[g], WT[g], pd], writes=[ST[g]])
                for g in range(4):
                    U = OSV[g][b]
                    Un = OSV[g][1 - b]
                    pso = OB[g // 2][0:4, (g % 2) * 64:(g % 2) * 64 + 64]
                    psd = OB[g // 2]
                    m.mm(pso, KKR[g][:, tc, :], ST[g][:], reads=[KKR[g], ST[g]], writes=[psd])
                    m.copy("act", U[0:4, tl + 1, :], pso, reads=[psd], writes=[U.d("os")])
                    if tl == RTC - 1:
                        m.copy("act", Un[0:4, 0, :], pso, reads=[psd], writes=[Un.d("os")])
            for g in range(4):
                U = OSV[g][b]
                m.dma("act", TO[ts0:ts0 + RTC, g * 128:(g + 1) * 128].rearrange("t (h k) -> h t k", h=2), U[0:2, 1:RTC + 1, :],
                      reads=[U.d("os")], writes=[TOd])
        for g in range(4 if globals().get("RW_STAGE", 3) >= 3 else 0):
            for s in range(RCH // 128):
                tsl = slice(t0 + s * 128, t0 + (s + 1) * 128)
                m.dma("sp", ot_s[:], TO[tsl, g * 128:(g + 1) * 128], reads=[TOd], writes=[ot_s])
                P = pp.next()
                m.tr(P[:, 0:128], ot_s[:], ident[:], reads=[ot_s, ident], writes=[P])
                m.copy("act", OT[:, s * 128:(s + 1) * 128], P[:, 0:128], reads=[P], writes=[OT])
            P = pp.next()
            m.mm(P[:, 0:RCH], BONES[:], OT[:], reads=[BONES, OT], writes=[P])
            m.stt("dve", cen[:], P[:, 0:RCH], -1.0 / 64.0, OT[:], ALU.mult, ALU.add, reads=[P, OT], writes=[cen])
            m.tt("dve", sq[:], cen[:], cen[:], ALU.mult, reads=[cen], writes=[sq])
            P = pp.next()
            m.mm(P[:, 0:RCH], BONES[:], sq[:], reads=[BONES, sq], writes=[P])
            m.ts("dve", sq[:], P[:, 0:RCH], 1.0 / 64.0, LN_EPS, ALU.mult, ALU.add, reads=[P], writes=[sq])
            m.act(sq[:], sq[:], AF.Sqrt, reads=[sq], writes=[sq])
            m.op("dve", lambda e: e.reciprocal(sq[:], sq[:]), reads=[sq], writes=[sq])
            m.tt("dve", cen[:], cen[:], sq[:], ALU.mult, reads=[cen, sq], writes=[cen])
            m.ts("dve", cen[:], cen[:], C["lnw"][:, g:g + 1], C["lnb"][:, g:g + 1], ALU.mult, ALU.add,
                 reads=[cen, C["lnw"], C["lnb"]], writes=[cen])
            m.tt("dve", cen[:], cen[:], BON[g][:], ALU.add, reads=[cen, BON[g]], writes=[cen])
            m.tt("dve", cen[:], cen[:], GTt[g][:], ALU.mult, reads=[cen, GTt[g]], writes=[cen])
            m.dma("act", io["ycT"][g * 128:(g + 1) * 128, t0:t0 + RCH], cen[:], reads=[cen], writes=[io["yc_dep"]])
    m.pop()

TTB = 256
NTB = 2048


def norm_src(m, src_ap, src_dep, hT_view, Gs, SHs, st, pp):
    xt, junk, ssq8, ssq, ident = st["xt"], st["junk"], st["ssq8"], st["ssq"], st["ident"]
    m.memset("dve", ssq8[:], 0.0, writes=[ssq8])
    for i in range(8):
        m.act(junk[:], src_ap[:, i * 512:(i + 1) * 512], AF.Square, reads=[src_dep, ssq8], writes=[junk, ssq8],
              accum_out=ssq8[:, i:i + 1])
    m.op("dve", lambda e: e.reduce_sum(ssq[:, 0:1], ssq8[:], AX.X), reads=[ssq8], writes=[ssq])
    rstd_inplace(m, ssq)
    m.ts("dve", xt[:], src_ap, ssq[:, 0:1], None, ALU.mult, reads=[src_dep, ssq], writes=[xt])
    for k4 in range(8):
        P = pp.next()
        for i in range(4):
            kc = k4 * 4 + i
            m.tr(P[:, i * 128:(i + 1) * 128], xt[:, kc * 128:(kc + 1) * 128], ident[:], reads=[xt, ident], writes=[P])
        for i in range(4):
            kc = k4 * 4 + i
            out_ap, out_dep = hT_view(kc)
            m.act(out_ap, P[:, i * 128:(i + 1) * 128], AF.Identity, reads=[P, Gs, SHs], writes=[out_dep],
                  scale=Gs[:, kc:kc + 1], bias=SHs[:, kc:kc + 1])


def build_B(last, ntiles=NTB // TTB, n_exp=32, dbg=False):
    nc = bass.Bass("TRN2", target_bir_lowering=False)

    def din(name, shape):
        return nc.dram_tensor(name, list(shape), F32, kind="ExternalInput").ap()
    x = din("x", [NTB, D])
    modall = din("modall", [128, 6, 32])
    adat = din("adat", [128, 6, 32])
    modrow = din("modrow", [6, D])
    adatrow = din("adatrow", [6, D])
    ng1 = din("ng1", [128, 32])
    ng2 = din("ng2", [128, 32])
    fgrow = din("fgrow", [1, D])
    yaT = din("yaT", [1024, NTB])
    yb = din("yb", [NTB, 2048])
    ycT = din("ycT", [1024, NTB])
    wm = din("wm", [96, 128, 4096])
    wbr = din("wbr", [32, 128, 4096])
    wout = din("wout", [32, 128, 4096])
    rw = din("rw", [128, 32, 32])
    rb = din("rb", [1, 32])
    wg = din("wg", [128, 128, 4096])
    wu = din("wu", [128, 128, 4096])
    bg = din("bg", [128, 32, 4])
    bu = din("bu", [128, 32, 4])
    wd = din("wd", [32, 8, 128, 2048])
    bd = din("bd", [32, D])
    ident_d = din("ident", [128, 128])
    xo = nc.dram_tensor("xo", [NTB, D], F32, kind="ExternalOutput").ap()
    xn = nc.dram_tensor("xn", [NTB, D], F32, kind="ExternalOutput").ap()
    dbg_m = nc.dram_tensor("dbg_m", [D, TTB], F32, kind="ExternalOutput").ap() if dbg else None
    dbg_x1 = nc.dram_tensor("dbg_x1", [TTB, D], F32, kind="ExternalOutput").ap() if dbg else None
    XO = Dep("xo")
    DBG = Dep("dbg")

    m = MK(nc)
    pp = PsumPool(m, 8)
    st = {}
    st["ident"] = m.sb("ident", [128, 128])
    m.dma("sp", st["ident"][:], ident_d, writes=[st["ident"]])
    st["xt"] = m.sb("xt", [128, D])
    st["junk"] = m.sb("junk", [128, 512])
    st["ssq8"] = m.sb("ssq8", [128, 8])
    st["ssq"] = m.sb("ssq", [128, 1])
    xt = st["xt"]
    mod = m.sb("mod", [128, 6, 32])
    tab = m.sb("tab", [128, 6, 32])
    n1 = m.sb("n1", [128, 32])
    n2 = m.sb("n2", [128, 32])
    m.dma("sp", mod[:], modall, writes=[mod])
    m.dma("sp", tab[:], adat, writes=[tab])
    m.dma("sp", n1[:], ng1, writes=[n1])
    m.dma("sp", n2[:], ng2, writes=[n2])
    m.tt("dve", mod[:], mod[:], tab[:], ALU.add, reads=[mod, tab], writes=[mod])
    G1 = m.sb("G1", [128, 32]); SH1 = m.sb("SH1", [128, 32]); G2 = m.sb("G2", [128, 32]); SH2 = m.sb("SH2", [128, 32])
    for (G, SH, ng, o) in ((G1, SH1, n1, 0), (G2, SH2, n2, 3)):
        m.ts("dve", G[:], mod[:, o + 1, :], 1.0, None, ALU.add, reads=[mod], writes=[G])
        m.tt("dve", G[:], G[:], ng[:], ALU.mult, reads=[G, ng], writes=[G])
        m.copy("dve", SH[:], mod[:, o + 0, :], reads=[mod], writes=[SH])
    ROW = m.sb("ROW", [128, D])

    def load_gate_row(i):
        m.dma("pool", ROW[:], modrow[i].partition_broadcast(128), writes=[ROW])
        m.dma("pool", xt[:], adatrow[i].partition_broadcast(128), writes=[xt])
        m.tt("dve", ROW[:], ROW[:], xt[:], ALU.add, reads=[ROW, xt], writes=[ROW])

    hT = m.sb("hT", [128, KC, TTB])
    YX = m.sb("YX", [128, 8192])
    MA = m.sb("MA", [128, 8192])
    YT = YX[:].rearrange("p (c t) -> p c t", t=TTB)
    X1 = YX[:].rearrange("p (s d) -> p s d", d=D)
    MT = MA[:].rearrange("p (c t) -> p c t", t=TTB)
    ACC = MA[:].rearrange("p (s d) -> p s d", d=D)
    WB = [m.sb(f"WB{i}", [128, 4096]) for i in range(2)]
    WD = [m.sb(f"WD{i}", [128, 2048]) for i in range(2)]
    ACTT = m.sb("ACTT", [128, 4, TTB])
    ybs = xt
    gs = m.sb("gs", [128, TTB])
    tmp = m.sb("tmp", [128, TTB])
    t_g = m.sb("t_g", [128, TTB]); t_s = m.sb("t_s", [128, TTB]); t_u = m.sb("t_u", [128, TTB])
    RW = m.sb("RW", [128, 32, 32]); RB = m.sb("RB", [1, 32])
    BG = m.sb("BG", [128, 32, 4]); BU = m.sb("BU", [128, 32, 4])
    BD = [m.sb(f"BD{i}", [1, 512]) for i in range(2)]
    ones = m.sb("ones1", [1, 128])
    lg = m.sb("lg", [128, 32]); m8 = m.sb("m8", [128, 8]); ex = m.sb("ex", [128, 32]); msk = m.sb("msk", [128, 32])
    sm = m.sb("sm", [128, 1]); WTS = m.sb("WTS", [128, 2, 32])
    m.dma("pool", RW[:], rw, writes=[RW]); m.dma("pool", RB[:], rb, writes=[RB])
    m.dma("pool", BG[:], bg, writes=[BG]); m.dma("pool", BU[:], bu, writes=[BU])
    m.memset("dve", ones[:], 1.0, writes=[ones])
    wi = [0]

    def wload(src):
        W = WB[wi[0] % 2]
        wi[0] += 1
        m.dma("sp", W[:], src, writes=[W])
        return W
    di = [0]

    for tt in range(ntiles):
        t0 = tt * TTB
        for s in range(2):
            norm_T(m, x[t0 + s * 128:t0 + (s + 1) * 128, :], lambda kc, s=s: (hT[:, kc, s * 128:(s + 1) * 128], hT),
                   G1, SH1, st, pp)
        m.dma("pool", YT[:, 0:8, :], yaT.rearrange("(c p) t -> p c t", p=128)[:, :, t0:t0 + TTB], writes=[YX])
        m.dma("pool", YT[:, 24:32, :], ycT.rearrange("(c p) t -> p c t", p=128)[:, :, t0:t0 + TTB], writes=[YX])
        for s in range(2):
            m.dma("pool", ybs[:, 0:2048], yb[t0 + s * 128:t0 + (s + 1) * 128, :], writes=[ybs])
            for c4 in range(4):
                P = pp.next()
                for i in range(4):
                    c = c4 * 4 + i
                    m.tr(P[:, i * 128:(i + 1) * 128], ybs[:, c * 128:(c + 1) * 128], st["ident"][:], reads=[ybs, st["ident"]], writes=[P])
                m.copy("act", YT[:, 8 + c4 * 4:8 + c4 * 4 + 4, s * 128:(s + 1) * 128],
                       P[:].rearrange("p (c t) -> p c t", t=128), reads=[P], writes=[YX])
        for dc in range(32):
            Wb = wload(wbr[dc])
            Pbr = []
            for (k0, k1) in ((0, 8), (8, 24), (24, 32)):
                P = pp.next()
                for kc in range(k0, k1):
                    m.mm(P[:, 0:TTB], Wb[:, kc * 128:(kc + 1) * 128], YT[:, kc, :], start=(kc == k0), stop=(kc == k1 - 1),
                         reads=[Wb, YX], writes=[P])
                Pbr.append(P)
            for br in range(3):
                Wg_ = wload(wm[br * 32 + dc])
                P = pp.next()
                for kc in range(KC):
                    m.mm(P[:, 0:TTB], Wg_[:, kc * 128:(kc + 1) * 128], hT[:, kc, :], start=(kc == 0), stop=(kc == KC - 1),
                         reads=[Wg_, hT], writes=[P])
                m.act(gs[:], P[:, 0:TTB], AF.Sigmoid, reads=[P], writes=[gs])
                if br == 0:
                    m.tt("dve", MT[:, dc, :], gs[:], Pbr[br][:, 0:TTB], ALU.mult, reads=[gs, Pbr[br]], writes=[MA])
                else:
                    m.tt("dve", tmp[:], gs[:], Pbr[br][:, 0:TTB], ALU.mult, reads=[gs, Pbr[br]], writes=[tmp])
                    m.tt("dve", MT[:, dc, :], MT[:, dc, :], tmp[:], ALU.add, reads=[MA, tmp], writes=[MA])
        if dbg and tt == 0:
            m.dma("act", dbg_m.rearrange("(c p) t -> p c t", p=128), MT, reads=[MA], writes=[DBG])
        load_gate_row(2)
        for s in range(2):
            m.dma("pool", X1[:, s, :], x[t0 + s * 128:t0 + (s + 1) * 128, :], writes=[YX])
        for cb in range(32):
            Wo = wload(wout[cb])
            for s in range(2):
                P = pp.next()
                for kc in range(KC):
                    m.mm(P[:, 0:128], MT[:, kc, s * 128:(s + 1) * 128], Wo[:, kc * 128:(kc + 1) * 128],
                         start=(kc == 0), stop=(kc == KC - 1), reads=[MA, Wo], writes=[P])
                m.tt("dve", tmp[:, 0:128], P[:, 0:128], ROW[:, cb * 128:(cb + 1) * 128], ALU.mult, reads=[P, ROW], writes=[tmp])
                m.tt("dve", X1[:, s, cb * 128:(cb + 1) * 128], X1[:, s, cb * 128:(cb + 1) * 128], tmp[:, 0:128], ALU.add,
                     reads=[YX, tmp], writes=[YX])
        if dbg and tt == 0:
            for s in range(2):
                m.dma("act", dbg_x1[s * 128:(s + 1) * 128, :], X1[:, s, :], reads=[YX], writes=[DBG])
        for s in range(2):
            norm_src(m, X1[:, s, :], YX, lambda kc, s=s: (hT[:, kc, s * 128:(s + 1) * 128], hT), G2, SH2, st, pp)
        for s in range(2):
            P = pp.next()
            for kc in range(KC):
                m.mm(P[:, 0:32], hT[:, kc, s * 128:(s + 1) * 128], RW[:, kc, :], start=(kc == 0), stop=False,
                     reads=[hT, RW], writes=[P])
            m.mm(P[:, 0:32], ones[:], RB[:], start=False, stop=True, reads=[ones, RB], writes=[P])
            m.copy("dve", lg[:], P[:, 0:32], reads=[P], writes=[lg])
            m.op("dve", lambda e: e.max(m8[:], lg[:]), reads=[lg], writes=[m8])
            m.ts("dve", msk[:], lg[:], m8[:, 3:4], None, ALU.is_ge, reads=[lg, m8], writes=[msk])
            m.ts("dve", ex[:], lg[:], m8[:, 0:1], None, ALU.subtract, reads=[lg, m8], writes=[ex])
            m.act(ex[:], ex[:], AF.Exp, reads=[ex], writes=[ex])
            m.tt("dve", ex[:], ex[:], msk[:], ALU.mult, reads=[ex, msk], writes=[ex])
            m.op("dve", lambda e: e.reduce_sum(sm[:, 0:1], ex[:], AX.X), reads=[ex], writes=[sm])
            m.op("dve", lambda e: e.reciprocal(sm[:, 0:1], sm[:, 0:1]), reads=[sm], writes=[sm])
            m.ts("dve", WTS[:, s, :], ex[:], sm[:, 0:1], None, ALU.mult, reads=[ex, sm], writes=[WTS])
        m.memset("pool", MA[:], 0.0, writes=[MA])
        for e_ in range(n_exp):
            for fc in range(4):
                Wg_ = wload(wg[e_ * 4 + fc])
                Pg = pp.next()
                for kc in range(KC):
                    m.mm(Pg[:, 0:TTB], Wg_[:, kc * 128:(kc + 1) * 128], hT[:, kc, :], start=(kc == 0), stop=(kc == KC - 1),
                         reads=[Wg_, hT], writes=[Pg])
                Wu_ = wload(wu[e_ * 4 + fc])
                Pu = pp.next()
                for kc in range(KC):
                    m.mm(Pu[:, 0:TTB], Wu_[:, kc * 128:(kc + 1) * 128], hT[:, kc, :], start=(kc == 0), stop=(kc == KC - 1),
                         reads=[Wu_, hT], writes=[Pu])
                m.ts("dve", t_g[:], Pg[:, 0:TTB], BG[:, e_, fc:fc + 1], 7.0, ALU.add, ALU.min, reads=[Pg, BG], writes=[t_g])
                m.act(t_s[:], t_g[:], AF.Sigmoid, reads=[t_g], writes=[t_s], scale=1.702)
                m.ts("dve", t_u[:], Pu[:, 0:TTB], BU[:, e_, fc:fc + 1], 7.0, ALU.add, ALU.min, reads=[Pu, BU], writes=[t_u])
                m.ts("dve", t_u[:], t_u[:], -7.0, 1.0, ALU.max, ALU.add, reads=[t_u], writes=[t_u])
                m.tt("dve", t_g[:], t_g[:], t_s[:], ALU.mult, reads=[t_g, t_s], writes=[t_g])
                m.tt("dve", ACTT[:, fc, :], t_g[:], t_u[:], ALU.mult, reads=[t_g, t_u], writes=[ACTT])
            for cb in range(8):
                Wd_ = WD[di[0] % 2]
                Bd_ = BD[di[0] % 2]
                di[0] += 1
                m.dma("sp", Wd_[:], wd[e_, cb], writes=[Wd_])
                m.dma("pool", Bd_[:], bd[e_:e_ + 1, cb * 512:(cb + 1) * 512], writes=[Bd_])
                for s in range(2):
                    P = pp.next()
                    for fc in range(4):
                        m.mm(P[:, 0:512], ACTT[:, fc, s * 128:(s + 1) * 128], Wd_[:, fc * 512:(fc + 1) * 512],
                             start=(fc == 0), stop=False, reads=[ACTT, Wd_], writes=[P])
                    m.mm(P[:, 0:512], ones[:], Bd_[:], start=False, stop=True, reads=[ones, Bd_], writes=[P])
                    m.stt("dve", ACC[:, s, cb * 512:(cb + 1) * 512], P[:, 0:512], WTS[:, s, e_:e_ + 1],
                          ACC[:, s, cb * 512:(cb + 1) * 512], ALU.mult, ALU.add, reads=[P, WTS, MA], writes=[MA])
        load_gate_row(5)
        for s in range(2):
            m.tt("dve", ACC[:, s, :], ACC[:, s, :], ROW[:], ALU.mult, reads=[MA, ROW], writes=[MA])
            m.tt("pool", X1[:, s, :], X1[:, s, :], ACC[:, s, :], ALU.add, reads=[YX, MA], writes=[YX])
        for s in range(2):
            m.dma("act", xo[t0 + s * 128:t0 + (s + 1) * 128, :], X1[:, s, :], reads=[YX], writes=[XO])
        m.dma("pool", ROW[:], fgrow[0].partition_broadcast(128), writes=[ROW])
        ssq8, ssq, junk = st["ssq8"], st["ssq"], st["junk"]
        for s in range(2):
            m.memset("dve", ssq8[:], 0.0, writes=[ssq8])
            for i in range(8):
                m.act(junk[:], X1[:, s, i * 512:(i + 1) * 512], AF.Square, reads=[YX, ssq8], writes=[junk, ssq8],
                      accum_out=ssq8[:, i:i + 1])
            m.op("dve", lambda e: e.reduce_sum(ssq[:, 0:1], ssq8[:], AX.X), reads=[ssq8], writes=[ssq])
            rstd_inplace(m, ssq)
            m.stt("dve", X1[:, s, :], X1[:, s, :], ssq[:, 0:1], ROW[:], ALU.mult, ALU.mult, reads=[YX, ssq, ROW], writes=[YX])
        for s in range(2):
            m.dma("act", xn[t0 + s * 128:t0 + (s + 1) * 128, :], X1[:, s, :], reads=[YX], writes=[XO])
    finals = [XO] + ([DBG] if dbg else [])
    m.finish(finals)
    return nc, m


def weights_B(inp, l):
    w = inp["w_in"][l]
    zm0 = 3 * 1024 + 2048 + 3072 + 48 + 3360
    d = {
        "wm": blkfmt(w[:, zm0:zm0 + 3 * D]),
        "wbr": blkfmt(inp["w_branch"][l]),
        "wout": blkfmt(inp["w_out"][l]),
        "rw": np.ascontiguousarray(inp["router_w"][l].reshape(32, 128, 32).transpose(1, 0, 2)),
        "rb": np.ascontiguousarray(inp["router_b"][l][None, :]),
        "wg": np.concatenate([blkfmt(inp["exp_wg"][l][e]) for e in range(32)], axis=0),
        "wu": np.concatenate([blkfmt(inp["exp_wu"][l][e]) for e in range(32)], axis=0),
        "bg": np.ascontiguousarray(inp["exp_bg"][l].reshape(32, 4, 128).transpose(2, 0, 1)),
        "bu": np.ascontiguousarray(inp["exp_bu"][l].reshape(32, 4, 128).transpose(2, 0, 1)),
        "wd": np.ascontiguousarray(inp["exp_wd"][l].reshape(32, 4, 128, 8, 512).transpose(0, 3, 2, 1, 4)).reshape(32, 8, 128, 2048),
        "bd": np.ascontiguousarray(inp["exp_bd"][l]),
        "ng1": colfmt(inp["norm_g"][l, 0]), "ng2": colfmt(inp["norm_g"][l, 1]),
        "adat": modfmt(inp["ada_table"][l]), "adatrow": np.ascontiguousarray(inp["ada_table"][l]),
        "fgrow": np.ascontiguousarray(inp["final_g"][None, :]),
        "ident": np.eye(128, dtype=np.float32),
    }
    return d


def inputs_B(wB, x_cur, mod_all, yaT_full, yb_full, ycT_full, b, j):
    ts_ = slice(NTB * j, NTB * (j + 1))
    d = dict(wB)
    d["x"] = np.ascontiguousarray(x_cur[b][ts_])
    d["modall"] = modfmt(mod_all[b].reshape(6, D))
    d["modrow"] = np.ascontiguousarray(mod_all[b].reshape(6, D))
    d["yaT"] = np.ascontiguousarray(yaT_full[b][:, ts_])
    d["yb"] = np.ascontiguousarray(yb_full[b][ts_])
    d["ycT"] = np.ascontiguousarray(ycT_full[b][:, ts_])
    return d

import os as _os
import sys as _sys
import time as _time

_PROGS = {}


def _prog(key, fn):
    if key not in _PROGS:
        _PROGS[key] = fn()
    return _PROGS[key]


def _log(*a):
    print("[kernel]", *a, file=_sys.stderr, flush=True)


def kernel(**inp):
    t_start = _time.time()
    dump = _os.environ.get("MK_DUMP")
    inp = {k: np.asarray(v) for k, v in inp.items()}
    x_cur = inp["x"].astype(np.float32, copy=False)
    mod_all = run_L0(inp)
    _log("L0 done", round(_time.time() - t_start, 1))
    if dump:
        np.save(dump + "/mod_all.npy", mod_all)
    cosT, sinT, rot = rope_consts()
    consts = {"ident": np.eye(128, dtype=np.float32), "rot": rot, "cosT": cosT, "sinT": sinT}
    consts.update(nsa_consts())
    cores = list(range(8))
    for l in range(2):
        ncA = _prog("A", lambda: build_A()[0])
        nsa_in = nsa_inputs(inp, l, consts)
        wcache = {}
        maps = []
        for core in cores:
            b, j = core // 2, core % 2
            if j not in wcache:
                wcache[j] = (weights_A(inp, l, j), rwkv_inputs(inp, l, j))
            d = inputs_A(inp, l, x_cur, mod_all, b, j, consts, wcache[j][0])
            d.update(nsa_in)
            d.update(wcache[j][1])
            maps.append(d)
        _log("A prep", l, round(_time.time() - t_start, 1))
        res = run_bass_kernel_spmd(ncA, maps, core_ids=cores).results
        _log("A done", l, round(_time.time() - t_start, 1))
        del maps, wcache
        yaT = [np.concatenate([res[2 * b]["yaT"], res[2 * b + 1]["yaT"]], axis=0) for b in range(4)]
        ycT = [np.concatenate([res[2 * b]["ycT"], res[2 * b + 1]["ycT"]], axis=0) for b in range(4)]
        yb = [np.concatenate([res[2 * b]["yb"], res[2 * b + 1]["yb"]], axis=1) for b in range(4)]
        del res
        if dump and l == 0:
            np.save(dump + "/yaT0.npy", yaT[0]); np.save(dump + "/yb0.npy", yb[0]); np.save(dump + "/ycT0.npy", ycT[0])
        ncB = _prog("B", lambda: build_B(False)[0])
        wB = weights_B(inp, l)
        maps = [inputs_B(wB, x_cur, mod_all, yaT, yb, ycT, core // 2, core % 2) for core in cores]
        _log("B prep", l, round(_time.time() - t_start, 1))
        res = run_bass_kernel_spmd(ncB, maps, core_ids=cores).results
        _log("B done", l, round(_time.time() - t_start, 1))
        del maps, wB
        key = "xn" if l == 1 else "xo"
        x_cur = np.stack([np.concatenate([res[2 * b][key], res[2 * b + 1][key]], axis=0) for b in range(4)], axis=0)
        del res
        if dump and l == 0:
            np.save(dump + "/x2_0.npy", x_cur[0])
    return x_cur.astype(np.float32, copy=False)
```
